# Optimizing a Trainium2 kernel written in Bass

```python
import math
import jax, jax.numpy as jnp
from jax import lax
import numpy as np

D_MODEL = 1024
BATCH = 16
SEQ = 2048
DEPTH = 4

N_MIXERS = 2
N_ATTN_LAYERS = (DEPTH + 1) // 2
N_RET_LAYERS = DEPTH // 2
NORM_EPS = 1e-6
N_NORMS = 6

D_FF = 2816

DILATED_GROUPS = ((128, 1), (512, 4), (2048, 16))
N_GROUPS = len(DILATED_GROUPS)
H_A = 16
DH_A = 64
D_A = H_A * DH_A
ATTN_IN_COLS = N_GROUPS * 3 * D_A
BLK = 64
NEG_INF = -1e30

NUM_BUCKETS = 32
REL_MAX_DISTANCE = 1024

H_R = 4
DK_R = D_MODEL // H_R
DV_R = 2 * D_MODEL // H_R
D_V = H_R * DV_R
RET_IN_COLS = 2 * H_R * DK_R + 3 * D_V
RET_CHUNK = 128
ROPE_BASE = 10000.0

kernel_name = "hybrid_dilated_attn_retention_macaron"


def rms_norm(x, g):
    xf = x.astype(jnp.float32)
    y = xf * lax.rsqrt(jnp.mean(xf * xf, axis=-1, keepdims=True) + NORM_EPS)
    return (y * g.astype(jnp.float32)).astype(x.dtype)


def swiglu(h, w_gate, w_up, w_down):
    return (jax.nn.silu(h @ w_gate) * (h @ w_up)) @ w_down


def t5_buckets(rel):
    half = NUM_BUCKETS // 2
    max_exact = half // 2
    n = np.abs(rel)
    large = max_exact + (np.log(np.maximum(n, 1) / max_exact)
                         / np.log(REL_MAX_DISTANCE / max_exact) * (half - max_exact)).astype(np.int64)
    large = np.minimum(large, half - 1)
    return ((rel > 0) * half + np.where(n < max_exact, n, large)).astype(np.int32)


def banded_attention(q, k, v, bias, radius):
    N, L, H, dh = q.shape
    nb = -(-L // BLK)
    Lp = nb * BLK
    qb = jnp.pad(q, ((0, 0), (0, Lp - L), (0, 0), (0, 0))).reshape(N, nb, BLK, H, dh)

    def windows(t):
        tp = jnp.pad(t, ((0, 0), (BLK, Lp - L + BLK), (0, 0), (0, 0))).reshape(N, nb + 2, BLK, H, dh)
        return jnp.concatenate([tp[:, :-2], tp[:, 1:-1], tp[:, 2:]], axis=2)

    kw, vw = windows(k), windows(v)
    a = np.arange(BLK)[:, None]
    c = np.arange(3 * BLK)[None, :]
    off = c - BLK - a
    key_pos = np.arange(nb)[:, None, None] * BLK - BLK + c[None]
    valid = (np.abs(off) <= radius)[None] & (key_pos >= 0) & (key_pos < L)
    bias_blk = bias[:, np.clip(off + radius, 0, 2 * radius)].astype(jnp.float32)

    s = jnp.einsum('nbqhd,nbkhd->nbhqk', qb, kw).astype(jnp.float32) * (dh ** -0.5) + bias_blk
    s = jnp.where(valid[None, :, None], s, NEG_INF)
    smax = jnp.max(s, axis=-1, keepdims=True)
    p = jnp.exp(s - smax)
    denom = jnp.sum(p, axis=-1, keepdims=True)
    o = jnp.einsum('nbhqk,nbkhd->nbqhd', (p / denom).astype(v.dtype), vw)
    lse = (smax + jnp.log(denom))[..., 0]
    o = o.reshape(N, Lp, H, dh)[:, :L]
    lse = lse.transpose(0, 1, 3, 2).reshape(N, Lp, H)[:, :L]
    return o, lse


def dilated_group(q, k, v, bias, dilation, radius):
    B, S, H, dh = q.shape
    Ls = S // dilation

    def split(t):
        return t.reshape(B, Ls, dilation, H, dh).transpose(0, 2, 1, 3, 4).reshape(B * dilation, Ls, H, dh)

    o, lse = banded_attention(split(q), split(k), split(v), bias, radius)
    o = o.reshape(B, dilation, Ls, H, dh).transpose(0, 2, 1, 3, 4).reshape(B, S, H, dh)
    lse = lse.reshape(B, dilation, Ls, H).transpose(0, 2, 1, 3).reshape(B, S, H)
    return o, lse


def dilated_attention(h, w_in, w_out, rel_bias):
    B, S, _ = h.shape
    proj = (h @ w_in).reshape(B, S, N_GROUPS, 3, H_A, DH_A)
    outs, lses = [], []
    for g, (window, dilation) in enumerate(DILATED_GROUPS):
        radius = window // (2 * dilation)
        buckets = t5_buckets(np.arange(-radius, radius + 1) * dilation)
        bias_g = rel_bias[g * H_A:(g + 1) * H_A][:, buckets]
        o, lse = dilated_group(proj[:, :, g, 0], proj[:, :, g, 1], proj[:, :, g, 2],
                               bias_g, dilation, radius)
        outs.append(o)
        lses.append(lse)
    wts = jax.nn.softmax(jnp.stack(lses, axis=0), axis=0)
    o = jnp.einsum('gbsh,gbshd->bshd', wts.astype(h.dtype), jnp.stack(outs, axis=0))
    return o.reshape(B, S, D_A) @ w_out


def rotary(t, pos):
    half = t.shape[-1] // 2
    inv_freq = 1.0 / (ROPE_BASE ** jnp.linspace(0.0, 1.0, half, dtype=jnp.float32))
    ang = pos[:, None] * inv_freq[None, :]
    cos = jnp.cos(ang)[None, :, None, :]
    sin = jnp.sin(ang)[None, :, None, :]
    t1, t2 = t[..., :half], t[..., half:]
    return jnp.concatenate([t1 * cos - t2 * sin, t1 * sin + t2 * cos], axis=-1)


def chunk_retention(q, k, v, log_gamma):
    B, S, H, dk = q.shape
    dv = v.shape[-1]
    C = RET_CHUNK
    n = S // C

    def to_chunks(t):
        return t.reshape(B, n, C, H, t.shape[-1]).transpose(1, 0, 3, 2, 4)

    qc, kc, vc = to_chunks(q), to_chunks(k), to_chunks(v)
    i = jnp.arange(C, dtype=jnp.float32)
    rel = i[:, None] - i[None, :]
    dmat = jnp.where(rel >= 0, jnp.exp(log_gamma[:, None, None] * jnp.maximum(rel, 0.0)), 0.0)
    inner = jnp.einsum('nbhid,nbhjd->nbhij', qc, kc) * dmat
    inner = jnp.einsum('nbhij,nbhje->nbhie', inner, vc)
    q_decay = jnp.exp(log_gamma[:, None] * (i + 1.0))[None, :, :, None]
    k_decay = jnp.exp(log_gamma[:, None] * (C - 1.0 - i))[None, :, :, None]
    chunk_decay = jnp.exp(log_gamma * C)[None, :, None, None]

    def step(state, xs):
        qi, ki, vi = xs
        cross = jnp.einsum('bhid,bhde->bhie', qi, state) * q_decay
        state = state * chunk_decay + jnp.einsum('bhjd,bhje->bhde', ki * k_decay, vi)
        return state, cross

    _, cross = lax.scan(step, jnp.zeros((B, H, dk, dv), jnp.float32), (qc, kc, vc))
    y = inner + cross
    return y.transpose(1, 0, 3, 2, 4).reshape(B, S, H, dv)


def head_group_norm(y):
    mu = jnp.mean(y, axis=-1, keepdims=True)
    var = jnp.mean(jnp.square(y - mu), axis=-1, keepdims=True)
    return (y - mu) * lax.rsqrt(var + NORM_EPS)


def retention(h, w_in, w_out, decay_logit):
    B, S, _ = h.shape
    proj = (h @ w_in).astype(jnp.float32)
    dq = H_R * DK_R
    q = proj[..., :dq].reshape(B, S, H_R, DK_R)
    k = proj[..., dq:2 * dq].reshape(B, S, H_R, DK_R)
    v = proj[..., 2 * dq:2 * dq + D_V].reshape(B, S, H_R, DV_R)
    g_f = proj[..., 2 * dq + D_V:2 * dq + 2 * D_V].reshape(B, S, H_R, DV_R)
    g_b = proj[..., 2 * dq + 2 * D_V:].reshape(B, S, H_R, DV_R)
    pos = jnp.arange(S, dtype=jnp.float32)
    q = rotary(q, pos)
    k = rotary(k, pos) * (DK_R ** -0.5)
    log_gamma = jnp.log1p(-jnp.exp(decay_logit.astype(jnp.float32)))
    y_f = chunk_retention(q, k, v, log_gamma[0])
    y_b = jnp.flip(chunk_retention(jnp.flip(q, 1), jnp.flip(k, 1), jnp.flip(v, 1), log_gamma[1]), 1)
    y = jax.nn.silu(g_f) * head_group_norm(y_f) + jax.nn.silu(g_b) * head_group_norm(y_b)
    return y.reshape(B, S, D_V).astype(h.dtype) @ w_out


def setup_inputs(seed: int = 0) -> dict:
    key = jax.random.key(seed)
    ks = jax.random.split(key, 12)
    f32 = jnp.float32
    x = jax.random.normal(ks[0], (BATCH, SEQ, D_MODEL), f32)
    norm_gains = 1.0 + 0.05 * jax.random.normal(ks[1], (DEPTH, N_NORMS, D_MODEL), f32)
    ffn_w_gate = jax.random.normal(ks[2], (DEPTH, 2, D_MODEL, D_FF), f32) * D_MODEL ** -0.5
    ffn_w_up = jax.random.normal(ks[3], (DEPTH, 2, D_MODEL, D_FF), f32) * D_MODEL ** -0.5
    ffn_w_down = jax.random.normal(ks[4], (DEPTH, 2, D_FF, D_MODEL), f32) * D_FF ** -0.5
    attn_w_in = jax.random.normal(ks[5], (N_ATTN_LAYERS, D_MODEL, ATTN_IN_COLS), f32) * D_MODEL ** -0.5
    attn_w_out = jax.random.normal(ks[6], (N_ATTN_LAYERS, D_A, D_MODEL), f32) * D_A ** -0.5
    rel_bias = 0.5 * jax.random.normal(ks[7], (N_GROUPS * H_A, NUM_BUCKETS), f32)
    ret_w_in = jax.random.normal(ks[8], (N_RET_LAYERS, D_MODEL, RET_IN_COLS), f32) * D_MODEL ** -0.5
    ret_w_out = jax.random.normal(ks[9], (N_RET_LAYERS, D_V, D_MODEL), f32) * D_V ** -0.5
    base = -(5.0 + jnp.arange(H_R, dtype=f32)) * math.log(2.0)
    ret_decay_logit = base[None, None, :] + 0.1 * jax.random.normal(ks[10], (N_RET_LAYERS, 2, H_R), f32)
    return {"x": x, "norm_gains": norm_gains, "ffn_w_gate": ffn_w_gate, "ffn_w_up": ffn_w_up,
            "ffn_w_down": ffn_w_down, "attn_w_in": attn_w_in, "attn_w_out": attn_w_out,
            "rel_bias": rel_bias, "ret_w_in": ret_w_in, "ret_w_out": ret_w_out,
            "ret_decay_logit": ret_decay_logit}


def reference(x, norm_gains, ffn_w_gate, ffn_w_up, ffn_w_down, attn_w_in, attn_w_out,
              rel_bias, ret_w_in, ret_w_out, ret_decay_logit):
    for i in range(DEPTH):
        g = norm_gains[i]
        h = swiglu(rms_norm(x, g[0]), ffn_w_gate[i, 0], ffn_w_up[i, 0], ffn_w_down[i, 0])
        x = x + 0.5 * rms_norm(h, g[1])
        hm = rms_norm(x, g[2])
        if i % N_MIXERS == 0:
            j = i // N_MIXERS
            m = dilated_attention(hm, attn_w_in[j], attn_w_out[j], rel_bias)
        else:
            j = i // N_MIXERS
            m = retention(hm, ret_w_in[j], ret_w_out[j], ret_decay_logit[j])
        x = x + rms_norm(m, g[3])
        h = swiglu(rms_norm(x, g[4]), ffn_w_gate[i, 1], ffn_w_up[i, 1], ffn_w_down[i, 1])
        x = x + 0.5 * rms_norm(h, g[5])
    return x
```

```python
import contextlib
import numpy as np
import concourse.bass as bass
import concourse.mybir as mybir
from concourse.bass_utils import run_bass_kernel_spmd

F32 = mybir.dt.float32
BF16 = mybir.dt.bfloat16
AF = mybir.ActivationFunctionType
ALU = mybir.AluOpType

D = 1024
S = 2048
DFF = 2816
NCH = DFF // 128
NL = 4
EPS = 1e-6
NCORES = 8
NSLOT = 4
DMA_K = 6


class Buf:
    __slots__ = ("name", "w", "r")

    def __init__(self, name):
        self.name = name
        self.w = None
        self.r = {}


class Sched:
    ENG = ("pe", "act", "dve", "pool", "sp")

    def __init__(self):
        self.streams = {e: [] for e in self.ENG}
        self.tick = {e: 0 for e in self.ENG}
        self.waited = {e: {} for e in self.ENG}
        self.pool_next = {q: 0 for q in ("sp", "pool", "act")}
        self.pool_target = {q: [0] * DMA_K for q in ("sp", "pool", "act")}

    def _waits(self, eng, reads, writes):
        need = {}

        def add(sem, val):
            if need.get(sem, 0) < val:
                need[sem] = val

        for b in reads:
            if b.w is not None:
                add(*b.w)
        for b in writes:
            if b.w is not None:
                add(*b.w)
            for sem, val in b.r.items():
                add(sem, val)
        out = []
        wd = self.waited[eng]
        for sem, val in need.items():
            if sem == "pe" and eng == "pe":
                continue
            if wd.get(sem, 0) >= val:
                continue
            wd[sem] = val
            out.append((sem, val))
        return out

    @staticmethod
    def _update(ev, reads, writes):
        sem, val = ev
        for b in reads:
            if b.r.get(sem, 0) < val:
                b.r[sem] = val
        for b in writes:
            b.w = ev
            b.r = {}

    def op(self, eng, fn, reads=(), writes=()):
        waits = self._waits(eng, reads, writes)
        self.tick[eng] += 1
        ev = (eng, self.tick[eng])
        self.streams[eng].append((waits, fn, ev, 1))
        self._update(ev, reads, writes)
        return ev

    def dma(self, q, fn, reads=(), writes=()):
        i = self.pool_next[q]
        self.pool_next[q] = (i + 1) % DMA_K
        semkey = ("dma", q, i)
        waits = self._waits(q, reads, writes)
        prev = self.pool_target[q][i]
        if prev > 0 and self.waited[q].get(semkey, 0) < prev:
            self.waited[q][semkey] = prev
            waits.append((semkey, prev))
        self.pool_target[q][i] = prev + 16
        ev = (semkey, prev + 16)
        self.streams[q].append((waits, fn, ev, 16))
        self._update(ev, reads, writes)
        return ev

    def sem_keys(self):
        keys = [e for e in self.ENG if e != "sp"]
        for q in ("sp", "pool", "act"):
            for i in range(DMA_K):
                keys.append(("dma", q, i))
        return keys

    def emit(self, nc, block, semh):
        final = []
        for q in ("sp", "pool", "act"):
            for i in range(DMA_K):
                if self.pool_target[q][i] > 0:
                    final.append((("dma", q, i), self.pool_target[q][i]))
        for e in ("pe", "act", "dve", "pool"):
            if self.tick[e] > 0:
                final.append((e, self.tick[e]))

        def run(eng, stream, tail=False):
            for waits, fn, ev, inc in stream:
                for sem, val in waits:
                    eng.wait_ge(semh[sem], val)
                ins = fn(eng)
                ins.then_inc(semh[ev[0]], inc)
            if tail:
                for sem, val in final:
                    eng.wait_ge(semh[sem], val)

        @block.tensor
        def _(pe):
            run(pe, self.streams["pe"])

        @block.scalar
        def _(act):
            run(act, self.streams["act"])

        @block.vector
        def _(dve):
            run(dve, self.streams["dve"])

        @block.gpsimd
        def _(pool):
            run(pool, self.streams["pool"])

        @block.sync
        def _(sp):
            run(sp, self.streams["sp"], tail=True)


class Ring:
    def __init__(self, items):
        self.items = items
        self.i = 0

    def next(self):
        it = self.items[self.i]
        self.i = (self.i + 1) % len(self.items)
        return it


class Arena:
    def __init__(self, ap, nbytes):
        self.ap = ap
        self.nbytes = nbytes
        self.off = 0
        self.marks = []

    def alloc(self, nbytes, dtype=BF16):
        nbytes = (nbytes + 63) // 64 * 64
        assert self.off + nbytes <= self.nbytes, ("SBUF arena overflow", self.off, nbytes, self.nbytes)
        a = self.ap[:, self.off // 2:(self.off + nbytes) // 2]
        self.off += nbytes
        if dtype == F32:
            a = a.bitcast(F32)
        return a

    def push(self):
        self.marks.append(self.off)

    def pop(self):
        self.off = self.marks.pop()


class Prog:
    def __init__(self, nseq, plan, debug_out=None):
        self.nseq = nseq
        self.plan = plan
        self.debug_out = debug_out
        self.dbg_names = []
        self.S = Sched()
        self.nc = bass.Bass("TRN2", target_bir_lowering=False)
        nc = self.nc
        T = nseq * S
        self.x_in = nc.dram_tensor("x", [T, D], F32, kind="ExternalInput").ap()
        self.y = nc.dram_tensor("y", [T, D], F32, kind="ExternalOutput").ap()
        self.gains = nc.dram_tensor("norm_gains", [NL, 6, D], F32, kind="ExternalInput").ap()
        self.wg = nc.dram_tensor("ffn_w_gate", [NL, 2, D, DFF], F32, kind="ExternalInput").ap()
        self.wu = nc.dram_tensor("ffn_w_up", [NL, 2, D, DFF], F32, kind="ExternalInput").ap()
        self.wd = nc.dram_tensor("ffn_w_down", [NL, 2, DFF, D], F32, kind="ExternalInput").ap()
        self.ident_d = nc.dram_tensor("ident", [128, 128], F32, kind="ExternalInput").ap()
        self.awin = nc.dram_tensor("attn_w_in", [2, D, 9216], F32, kind="ExternalInput").ap()
        self.awout = nc.dram_tensor("attn_w_out", [2, D, D], F32, kind="ExternalInput").ap()
        self.biasexp = nc.dram_tensor("biasexp", [128, 48, 256], F32, kind="ExternalInput").ap()
        self.rwin = nc.dram_tensor("ret_w_in", [2, D, 8192], F32, kind="ExternalInput").ap()
        self.rwout = nc.dram_tensor("ret_w_out", [2, 2048, D], F32, kind="ExternalInput").ap()
        self.rdl = nc.dram_tensor("ret_decay_logit", [2, 8], F32, kind="ExternalInput").ap()
        self.rconst = nc.dram_tensor("rconst", [128, 4 * 128 + 2], F32, kind="ExternalInput").ap()
        self.cs_d = nc.dram_tensor("cossin", [128, 2, S], F32, kind="ExternalInput").ap()
        self.yT_d = nc.dram_tensor("yT_scratch", [nseq, 128, 16, S], BF16, kind="Internal").ap()

    def debug(self, name, ap, reads):
        if self.debug_out is None or name not in self.debug_out:
            return
        shape = list(ap.shape)
        d = self.nc.dram_tensor("dbg_" + name, shape, F32, kind="ExternalOutput").ap()
        self.S.dma("pool", lambda e: e.dma_start(out=d, in_=ap), reads=reads)
        self.dbg_names.append("dbg_" + name)

    def bufs(self, name, n):
        return [Buf(f"{name}{i}") for i in range(n)]

    def build(self):
        nc = self.nc
        Sx = self.S
        with contextlib.ExitStack() as es:
            ARENA_BYTES = 207 * 1024
            arena_t = es.enter_context(nc.sbuf_tensor("arena", [128, ARENA_BYTES // 2], BF16))
            ps_t = es.enter_context(nc.psum_tensor("ps", [128, 8, 512], F32))
            self.ps = ps_t
            self.bank = self.bufs("bank", 8)
            A = Arena(arena_t, ARENA_BYTES)
            self.A = A
            self.ident = A.alloc(256)
            self.ident_b = Buf("ident")
            self.stat = A.alloc(16 * 16, F32)
            self.stat_ring = Ring([(self.stat[:, 4 * i:4 * i + 4], Buf(f"stat{i}")) for i in range(16)])
            self.gain = [(A.alloc(4096, F32), Buf(f"gain{i}")) for i in range(4)]
            self.xt_ring = Ring([(A.alloc(4096, F32), Buf(f"xt{i}")) for i in range(3)])
            self.hb_ring = Ring([(A.alloc(2048), Buf(f"hb{i}")) for i in range(2)])
            self.t1_ring = Ring([(A.alloc(4096, F32), Buf(f"t1_{i}")) for i in range(2)])
            self.hT = A.alloc(8 * S * 2).rearrange("p (k t) -> p k t", t=S)
            self.hT_b = self.bufs("hT", 16)
            self.wring = [(A.alloc(8192), Buf(f"wslot{i}")) for i in range(NSLOT)]
            self.x_b = [self.bufs(f"x{s}_", 16) for s in range(self.nseq)]
            self.yTd_b = [self.bufs(f"yTd{s}_", 4) for s in range(self.nseq)]
            self.x_first = True

            Sx.dma("pool", lambda e: e.dma_start(out=self.ident, in_=self.ident_d), writes=[self.ident_b])

            units = self.make_units()
            self.slabs = []
            for u in units:
                u["slab0"] = len(self.slabs)
                self.slabs.extend(u["slabs"])
            self.slab_issued = 0
            self.slab_next = 0
            for u in units:
                u["run"](u)

            sem_keys = Sx.sem_keys()
            semh = {}
            for i, k in enumerate(sem_keys):
                semh[k] = es.enter_context(nc.semaphore(f"s{i}"))
            block = es.enter_context(nc.Block())
            Sx.emit(nc, block, semh)
        return nc

    def w_issue_upto(self, idx):
        while self.slab_issued <= min(idx, len(self.slabs) - 1):
            i = self.slab_issued
            slot_ap, slot_b = self.wring[i % NSLOT]
            for (dst_fn, src) in self.slabs[i]:
                dst = dst_fn(slot_ap)
                self.S.dma("pool", (lambda d, s_: (lambda e: e.dma_start(out=d, in_=s_)))(dst, src), writes=[slot_b])
            self.slab_issued += 1

    def w_get(self, hold=0):
        i = self.slab_next
        self.slab_next += 1
        self.w_issue_upto(i + NSLOT - 1 - hold)
        return self.wring[i % NSLOT]

    def x_rows(self, first, s, t):
        src = self.x_in if first else self.y
        r0 = s * S + t * 128
        return src[r0:r0 + 128, :]

    def load_gain(self, slot, layer, idx):
        g_ap, g_b = self.gain[slot]
        src = self.gains[layer, idx:idx + 1, :].partition_broadcast(128)
        self.S.dma("sp", lambda e: e.dma_start(out=g_ap, in_=src), writes=[g_b])

    def rstd_ops(self, st, st_b, n):
        Sx = self.S
        Sx.op("act", lambda e: e.activation(out=st[:, 1:2], in_=st[:, 0:1], func=AF.Ln, scale=1.0 / n, bias=EPS),
              reads=[st_b], writes=[st_b])
        Sx.op("act", lambda e: e.activation(out=st[:, 2:3], in_=st[:, 1:2], func=AF.Exp, scale=-0.5),
              reads=[st_b], writes=[st_b])

    def prenorm_tile(self, first, s, t, gslot):
        Sx = self.S
        xs, xs_b = self.xt_ring.next()
        hb, hb_b = self.hb_ring.next()
        st, st_b = self.stat_ring.next()
        g_ap, g_b = self.gain[gslot]
        src = self.x_rows(first, s, t)
        Sx.dma("sp", lambda e: e.dma_start(out=xs, in_=src), reads=[self.x_b[s][t]], writes=[xs_b])
        Sx.op("act", lambda e: e.activation(out=hb, in_=xs, func=AF.Square, accum_out=st[:, 0:1]),
              reads=[xs_b], writes=[hb_b, st_b])
        self.rstd_ops(st, st_b, D)
        Sx.op("dve", lambda e: e.scalar_tensor_tensor(out=hb, in0=xs, scalar=st[:, 2:3], in1=g_ap,
                                                      op0=ALU.mult, op1=ALU.mult),
              reads=[xs_b, st_b, g_b], writes=[hb_b])
        pT = self.ps[:, 7, 0:512].bitcast(BF16).rearrange("p (k t) -> p k t", t=128)
        ident = self.ident

        def tr(pe):
            for k in range(8):
                ins = pe.transpose(pT[:, k, :], hb[:, k * 128:(k + 1) * 128], ident)
            return ins
        Sx.op("pe", tr, reads=[hb_b, self.ident_b], writes=[self.bank[7]])
        hT = self.hT
        Sx.op("act", lambda e: e.activation(out=hT[:, :, t * 128:(t + 1) * 128], in_=pT, func=AF.Copy),
              reads=[self.bank[7]], writes=[self.hT_b[t]])

    def postnorm_tile(self, first, s, t, b0, gslot, coef):
        Sx = self.S
        m = self.ps[:, b0:b0 + 2, :].rearrange("p a b -> p (a b)")
        mb = [self.bank[b0], self.bank[b0 + 1]]
        t1, t1_b = self.t1_ring.next()
        st, st_b = self.stat_ring.next()
        xs, xs_b = self.xt_ring.next()
        g_ap, g_b = self.gain[gslot]
        src = self.x_rows(first, s, t)
        dst = self.y[s * S + t * 128: s * S + t * 128 + 128, :]
        Sx.dma("sp", lambda e: e.dma_start(out=xs, in_=src), reads=[self.x_b[s][t]], writes=[xs_b])
        Sx.op("act", lambda e: e.activation(out=t1, in_=m, func=AF.Square, accum_out=st[:, 0:1]),
              reads=mb, writes=[t1_b, st_b])
        self.rstd_ops(st, st_b, D)
        Sx.op("dve", lambda e: e.scalar_tensor_tensor(out=t1, in0=m, scalar=st[:, 2:3], in1=g_ap,
                                                      op0=ALU.mult, op1=ALU.mult),
              reads=mb + [st_b, g_b], writes=[t1_b])
        Sx.op("dve", lambda e: e.scalar_tensor_tensor(out=xs, in0=t1, scalar=float(coef), in1=xs,
                                                      op0=ALU.mult, op1=ALU.add),
              reads=[t1_b, xs_b], writes=[xs_b])
        Sx.dma("sp", lambda e: e.dma_start(out=dst, in_=xs), reads=[xs_b], writes=[self.x_b[s][t]])

    def make_units(self):
        units = []
        for (layer, sub) in self.plan:
            if sub in (0, 2):
                f = 0 if sub == 0 else 1
                for s in range(self.nseq):
                    for b in range(2):
                        units.append(self.ffn_unit(layer, f, s, b))
            elif layer % 2 == 0:
                for s in range(self.nseq):
                    units.append(self.attn_unit(layer, s))
            else:
                for s in range(self.nseq):
                    units.append(self.ret_unit(layer, s))
        return units

    def ffn_unit(self, layer, f, s, b):
        wg, wu = self.wg[layer, f], self.wu[layer, f]
        slabs = []
        for j in range(NCH // 2):
            c0 = j * 256
            gsrc = wg[:, c0:c0 + 256].rearrange("(k p) n -> p k n", p=128)
            usrc = wu[:, c0:c0 + 256].rearrange("(k p) n -> p k n", p=128)
            slabs.append([
                (lambda sl: sl.rearrange("p (k n) -> p k n", n=512)[:, :, 0:256], gsrc),
                (lambda sl: sl.rearrange("p (k n) -> p k n", n=512)[:, :, 256:512], usrc),
            ])
        first = (layer, 0 if f == 0 else 2) == tuple(self.plan[0])
        return dict(kind="ffn", layer=layer, f=f, s=s, b=b, slabs=slabs, run=self.ffn_run, first=first)

    def ffn_alloc(self):
        A = self.A
        A.push()
        self.aT = A.alloc(NCH * 1024 * 2).rearrange("p (c t) -> p c t", t=1024)
        self.aT_b = self.bufs("aT", 2)
        self.Wd = A.alloc(NCH * 1024 * 2).rearrange("p (c n) -> p c n", n=1024)
        self.Wd_b = Buf("Wd")
        self.sg_ring = Ring([(A.alloc(2048, F32), Buf(f"sg{i}")) for i in range(2)])
        self.Wd_loaded = None
        A.pop()

    def ffn_run(self, u):
        Sx = self.S
        layer, f, s, b = u["layer"], u["f"], u["s"], u["b"]
        first = u["first"]
        if getattr(self, "phase", None) != "ffn":
            self.phase_switch("ffn")
        gset = 2 * ((layer * 3 + 2 * f) % 2)
        if s == 0 and b == 0:
            self.load_gain(gset, layer, 0 if f == 0 else 4)
            self.load_gain(gset + 1, layer, 1 if f == 0 else 5)
        for t in range(8 * b, 8 * b + 8):
            self.prenorm_tile(first, s, t, gset)
        hT, aT, Wd, ps = self.hT, self.aT, self.Wd, self.ps
        n = 0
        for j in range(NCH // 2):
            slot, slot_b = self.w_get()
            if j == 0 and self.Wd_loaded != (layer, f):
                self.Wd_loaded = (layer, f)
                wd = self.wd[layer, f]
                for q in range(2):
                    src = wd[q * 1408:(q + 1) * 1408, :].rearrange("(c p) n -> p c n", p=128)
                    dstw = Wd[:, q * 11:(q + 1) * 11, :]
                    Sx.dma("pool", (lambda d_, s_: (lambda e: e.dma_start(out=d_, in_=s_)))(dstw, src),
                           writes=[self.Wd_b])
            w3 = slot.rearrange("p (k n) -> p k n", n=512)
            for cc in range(2):
                c = 2 * j + cc
                for tb in range(2):
                    col0 = b * 1024 + tb * 512
                    G, U = n % 2, 2 + n % 2
                    n += 1
                    hbufs = self.hT_b[col0 // 128: col0 // 128 + 4]

                    def mmg(pe, off=cc * 128, col0=col0, bank=G, w3=w3):
                        for k in range(8):
                            ins = pe.matmul(ps[:, bank, :], lhsT=w3[:, k, off:off + 128], rhs=hT[:, k, col0:col0 + 512],
                                            start=(k == 0), stop=(k == 7))
                        return ins
                    Sx.op("pe", mmg, reads=[slot_b] + hbufs, writes=[self.bank[G]])

                    def mmu(pe, off=256 + cc * 128, col0=col0, bank=U, w3=w3):
                        for k in range(8):
                            ins = pe.matmul(ps[:, bank, :], lhsT=w3[:, k, off:off + 128], rhs=hT[:, k, col0:col0 + 512],
                                            start=(k == 0), stop=(k == 7))
                        return ins
                    Sx.op("pe", mmu, reads=[slot_b] + hbufs, writes=[self.bank[U]])
                    sg, sg_b = self.sg_ring.next()
                    Sx.op("act", lambda e, sg=sg, bank=G: e.activation(out=sg, in_=ps[:, bank, :], func=AF.Silu),
                          reads=[self.bank[G]], writes=[sg_b])
                    Sx.op("dve", lambda e, sg=sg, bank=U, c=c, tb=tb: e.tensor_tensor(
                        out=aT[:, c, tb * 512:(tb + 1) * 512], in0=sg, in1=ps[:, bank, :], op=ALU.mult),
                        reads=[sg_b, self.bank[U]], writes=[self.aT_b[tb]])
        if b == 0 and s == 0:
            self.debug("hT", hT[:, :, 0:1024], self.hT_b[0:8])
            self.debug("aT", aT, self.aT_b)
            self.debug("Wd", Wd, [self.Wd_b])
        for tt in range(8):
            b0 = 4 + 2 * (tt % 2)

            def mmd(pe, tt=tt, b0=b0):
                for h in range(2):
                    for c in range(NCH):
                        ins = pe.matmul(ps[:, b0 + h, :], lhsT=aT[:, c, tt * 128:(tt + 1) * 128],
                                        rhs=Wd[:, c, h * 512:(h + 1) * 512], start=(c == 0), stop=(c == NCH - 1))
                return ins
            Sx.op("pe", mmd, reads=[self.aT_b[tt // 4], self.Wd_b], writes=[self.bank[b0], self.bank[b0 + 1]])
            self.postnorm_tile(first, s, 8 * b + tt, b0, gset + 1, 0.5)

    def attn_unit(self, layer, s):
        j = layer // 2
        win = self.awin[j]
        slabs = []
        for p in range(8):
            for g in range(3):
                parts = []
                for qi in range(3):
                    c0 = g * 3072 + qi * 1024 + p * 128
                    src = win[:, c0:c0 + 128].rearrange("(k p) n -> p k n", p=128)
                    parts.append(((lambda sl, qi=qi: sl.rearrange("p (k n) -> p k n", n=512)[:, :, qi * 128:(qi + 1) * 128]), src))
                slabs.append(parts)
        wout = self.awout[j]
        for h in range(2):
            src = wout[:, h * 512:(h + 1) * 512].rearrange("(k p) n -> p k n", p=128)
            slabs.append([((lambda sl: sl.rearrange("p (k n) -> p k n", n=512)), src)])
        first = (layer, 1) == tuple(self.plan[0])
        return dict(kind="attn", layer=layer, s=s, slabs=slabs, run=self.attn_run, first=first)

    def attn_alloc(self):
        A = self.A
        A.push()
        self.oT = A.alloc(8 * S * 2).rearrange("p (k t) -> p k t", t=S)
        self.oT_b = Buf("oT")
        self.acc = [A.alloc(S * 4, F32), A.alloc(S * 4, F32)]
        self.acc_b = self.bufs("acc", 2)
        self.den = A.alloc(S * 4, F32)
        self.den_b = self.bufs("den", 2)
        self.qT = A.alloc(S * 2)
        self.kT = A.alloc(S * 2)
        self.qT_b, self.kT_b = Buf("qT"), Buf("kT")
        self.Vaug = [A.alloc(16 * 128 * 2).rearrange("p (c n) -> p c n", n=128) for _ in range(2)]
        self.Vaug_b = self.bufs("Vaug", 2)
        self.E = A.alloc(48 * 256 * 2).rearrange("p (g n) -> p g n", n=256)
        self.E_b = Buf("E")
        self.pe32_ring = Ring([(A.alloc(1024, F32), Buf(f"pe32_{i}")) for i in range(2)])
        self.PT_ring = Ring([(A.alloc(512), Buf(f"PT{i}")) for i in range(2)])
        A.pop()

    def attn_setup(self):
        Sx = self.S
        E = self.E
        for q in range(4):
            stage = self.acc[0] if q % 2 == 0 else self.acc[1]
            stage_b = self.acc_b[q % 2]
            st3 = stage.rearrange("p (g n) -> p g n", n=256)[:, 0:8, :]
            src = self.biasexp[:, q * 12:q * 12 + 8, :]
            Sx.dma("sp", lambda e, st3=st3, src=src: e.dma_start(out=st3, in_=src), writes=[stage_b])
            Sx.op("act", lambda e, st3=st3, q=q: e.activation(out=E[:, q * 12:q * 12 + 8, :], in_=st3, func=AF.Exp),
                  reads=[stage_b], writes=[self.E_b])
            st4 = stage.rearrange("p (g n) -> p g n", n=256)[:, 0:4, :]
            src2 = self.biasexp[:, q * 12 + 8:q * 12 + 12, :]
            Sx.dma("sp", lambda e, st4=st4, src2=src2: e.dma_start(out=st4, in_=src2), reads=[], writes=[stage_b])
            Sx.op("act", lambda e, st4=st4, q=q: e.activation(out=E[:, q * 12 + 8:q * 12 + 12, :], in_=st4, func=AF.Exp),
                  reads=[stage_b], writes=[self.E_b])
        for i in range(2):
            Sx.op("dve", lambda e, i=i: e.memset(self.Vaug[i], 1.0), writes=[self.Vaug_b[i]])

    def attn_run(self, u):
        Sx = self.S
        layer, s, first = u["layer"], u["s"], u["first"]
        if getattr(self, "phase", None) != "attn":
            self.phase_switch("attn")
        gset = 2 * ((layer * 3 + 1) % 2)
        if s == 0:
            self.load_gain(gset, layer, 2)
            self.load_gain(gset + 1, layer, 3)
        for t in range(16):
            self.prenorm_tile(first, s, t, gset)
        hT, ps, oT = self.hT, self.ps, self.oT
        qT, kT = self.qT, self.kT
        nproj = 0
        nv = 0
        nst = 0
        for p in range(8):
            for i in range(2):
                Sx.op("dve", lambda e, i=i: e.memset(self.acc[i], 0.0), writes=[self.acc_b[i]])
            for g in range(3):
                r = (1, 4, 16)[g]
                Ls = S // r
                nb = Ls // 128
                slot, slot_b = self.w_get()
                w3 = slot.rearrange("p (k n) -> p k n", n=512)
                for which, dstT, dst_b, scale in ((0, qT, self.qT_b, 0.125), (1, kT, self.kT_b, 1.0)):
                    for tb in range(4):
                        bank = nproj % 2
                        nproj += 1

                        def mmp(pe, w3=w3, off=which * 128, tb=tb, bank=bank):
                            for k in range(8):
                                ins = pe.matmul(ps[:, bank, :], lhsT=w3[:, k, off:off + 128],
                                                rhs=hT[:, k, tb * 512:(tb + 1) * 512], start=(k == 0), stop=(k == 7))
                            return ins
                        Sx.op("pe", mmp, reads=[slot_b] + self.hT_b[tb * 4:tb * 4 + 4], writes=[self.bank[bank]])
                        Sx.op("act", lambda e, dstT=dstT, tb=tb, bank=bank, scale=scale: e.activation(
                            out=dstT[:, tb * 512:(tb + 1) * 512], in_=ps[:, bank, :], func=AF.Copy, scale=scale),
                            reads=[self.bank[bank]], writes=[dst_b])
                hT4 = hT.rearrange("p k (t r) -> p k t r", r=r)
                for c4 in range(4):
                    bank = 2 + nv % 2
                    nv += 1
                    pv3 = ps[:, bank, :].rearrange("p (c n) -> p c n", n=128)

                    def mmv(pe, w3=w3, c4=c4, pv3=pv3, hT4=hT4, nb=nb):
                        for cl in range(4):
                            ci = c4 * 4 + cl
                            res, jb = ci // nb, ci % nb
                            for k in range(8):
                                ins = pe.matmul(pv3[:, cl, :], lhsT=hT4[:, k, jb * 128:(jb + 1) * 128, res],
                                                rhs=w3[:, k, 256:384], start=(k == 0), stop=(k == 7))
                        return ins
                    Sx.op("pe", mmv, reads=[slot_b] + self.hT_b, writes=[self.bank[bank]])
                    Sx.op("act", lambda e, c4=c4, pv3=pv3: e.activation(
                        out=self.Vaug[0][:, c4 * 4:c4 * 4 + 4, 0:64], in_=pv3[:, :, 0:64], func=AF.Copy),
                        reads=[self.bank[bank]], writes=[self.Vaug_b[0]])
                    Sx.op("act", lambda e, c4=c4, pv3=pv3: e.activation(
                        out=self.Vaug[1][:, c4 * 4:c4 * 4 + 4, 64:128], in_=pv3[:, :, 64:128], func=AF.Copy),
                        reads=[self.bank[bank]], writes=[self.Vaug_b[1]])
                q3 = qT.rearrange("p (t r) -> p t r", r=r)
                k3 = kT.rearrange("p (t r) -> p t r", r=r)
                work = [(e_, ci) for e_ in range(2) for ci in range(16)]
                pend = None

                def st_stage(e_, ci):
                    nonlocal nst
                    res, jb = ci // nb, ci % nb
                    qlo, qhi = max(0, 128 * jb - 64), min(Ls, 128 * jb + 192)
                    nq = qhi - qlo
                    qi0 = qlo - (128 * jb - 64)
                    bank = 4 + nst % 2
                    nst += 1
                    hp = slice(64 * e_, 64 * e_ + 64)
                    l_ap = k3[hp, jb * 128:(jb + 1) * 128, res]
                    r_ap = q3[hp, qlo:qhi, res]
                    Sx.op("pe", lambda pe, bank=bank: pe.matmul(
                        ps[:, bank, 0:nq], lhsT=l_ap, rhs=r_ap, start=True, stop=True),
                        reads=[self.qT_b, self.kT_b], writes=[self.bank[bank]])
                    pe32, pe32_b = self.pe32_ring.next()
                    PT, PT_b = self.PT_ring.next()
                    Sx.op("act", lambda e, bank=bank: e.activation(out=pe32[:, 0:nq], in_=ps[:, bank, 0:nq], func=AF.Exp),
                          reads=[self.bank[bank]], writes=[pe32_b])
                    gh = g * 16 + 2 * p + e_
                    e_ap = self.E[:, gh, qi0:qi0 + nq]
                    Sx.op("dve", lambda e: e.tensor_tensor(out=PT[:, 0:nq], in0=pe32[:, 0:nq], in1=e_ap, op=ALU.mult),
                          reads=[pe32_b, self.E_b], writes=[PT_b])
                    return (e_, ci, res, qlo, qhi, nq, PT, PT_b, bank)

                def pv_stage(stg):
                    e_, ci, res, qlo, qhi, nq, PT, PT_b, bank = stg
                    ob = bank + 2
                    v_ap = self.Vaug[e_][:, ci, :]
                    Sx.op("pe", lambda pe: pe.matmul(ps[:, ob, 0:nq], lhsT=v_ap, rhs=PT[:, 0:nq], start=True, stop=True),
                          reads=[self.Vaug_b[e_], PT_b], writes=[self.bank[ob]])
                    a3 = self.acc[e_].rearrange("p (t r) -> p t r", r=r)
                    Sx.op("dve", lambda e: e.tensor_tensor(out=a3[:, qlo:qhi, res], in0=a3[:, qlo:qhi, res],
                                                           in1=ps[:, ob, 0:nq], op=ALU.add),
                          reads=[self.bank[ob], self.acc_b[e_]], writes=[self.acc_b[e_]])

                for (e_, ci) in work:
                    stg = st_stage(e_, ci)
                    if pend is not None:
                        pv_stage(pend)
                    pend = stg
                pv_stage(pend)
            den = self.den
            Sx.dma("sp", lambda e: e.dma_start(out=den[0:64, :], in_=self.acc[0][64:128, :]),
                   reads=[self.acc_b[0]], writes=[self.den_b[0]])
            Sx.dma("sp", lambda e: e.dma_start(out=den[64:128, :], in_=self.acc[1][0:64, :]),
                   reads=[self.acc_b[1]], writes=[self.den_b[1]])
            Sx.op("dve", lambda e: e.reciprocal(out=den, in_=den), reads=self.den_b, writes=self.den_b)
            Sx.op("dve", lambda e, p=p: e.tensor_tensor(out=oT[0:64, p, :], in0=self.acc[0][0:64, :], in1=den[0:64, :],
                                                        op=ALU.mult),
                  reads=[self.acc_b[0]] + self.den_b, writes=[self.oT_b])
            Sx.op("dve", lambda e, p=p: e.tensor_tensor(out=oT[64:128, p, :], in0=self.acc[1][64:128, :],
                                                        in1=den[64:128, :], op=ALU.mult),
                  reads=[self.acc_b[1]] + self.den_b, writes=[self.oT_b])
        slots = [self.w_get(), self.w_get(hold=1)]
        for t in range(16):
            b0 = 2 * (t % 2)

            def mmo(pe, t=t, b0=b0):
                for h in range(2):
                    w3 = slots[h][0].rearrange("p (k n) -> p k n", n=512)
                    for k in range(8):
                        ins = pe.matmul(ps[:, b0 + h, :], lhsT=oT[:, k, t * 128:(t + 1) * 128], rhs=w3[:, k, :],
                                        start=(k == 0), stop=(k == 7))
                return ins
            Sx.op("pe", mmo, reads=[slots[0][1], slots[1][1], self.oT_b], writes=[self.bank[b0], self.bank[b0 + 1]])
            self.postnorm_tile(first, s, t, b0, gset + 1, 1.0)

    def ret_unit(self, layer, s):
        j = layer // 2
        win = self.rwin[j]
        full = lambda sl: sl.rearrange("p (k n) -> p k n", n=512)
        slabs = []
        for h in range(4):
            qsrc = win[:, h * 256:(h + 1) * 256].rearrange("(k p) n -> p k n", p=128)
            ksrc = win[:, 1024 + h * 256:1024 + (h + 1) * 256].rearrange("(k p) n -> p k n", p=128)
            slabs.append([((lambda sl: sl.rearrange("p (k n) -> p k n", n=512)[:, :, 0:256]), qsrc),
                          ((lambda sl: sl.rearrange("p (k n) -> p k n", n=512)[:, :, 256:512]), ksrc)])
            for base in (2048, 4096, 6144):
                src = win[:, base + h * 512: base + (h + 1) * 512].rearrange("(k p) n -> p k n", p=128)
                slabs.append([(full, src)])
        wout = self.rwout[j]
        for dh in range(2):
            for kh in range(2):
                src = wout[kh * 1024:(kh + 1) * 1024, dh * 512:(dh + 1) * 512].rearrange("(k p) n -> p k n", p=128)
                slabs.append([(full, src)])
        first = (layer, 1) == tuple(self.plan[0])
        return dict(kind="ret", layer=layer, s=s, slabs=slabs, run=self.ret_run, first=first)

    def ret_alloc(self):
        A = self.A
        A.push()
        self.cs = A.alloc(2 * S * 4, F32).rearrange("p (a t) -> p a t", t=S)
        self.cs_b = Buf("cs")
        self.QrT = A.alloc(2 * S * 2).rearrange("p (a t) -> p a t", t=S)
        self.KrT = A.alloc(2 * S * 2).rearrange("p (a t) -> p a t", t=S)
        self.QrT_b, self.KrT_b = Buf("QrT"), Buf("KrT")
        self.Krtm = A.alloc(16 * 256 * 2).rearrange("p (c n) -> p c n", n=256)
        self.Krtm_b = Buf("Krtm")
        self.Vtm = A.alloc(16 * 512 * 2).rearrange("p (c n) -> p c n", n=512)
        self.Vtm_b = Buf("Vtm")
        self.YF = A.alloc(16 * 512 * 2).rearrange("p (c n) -> p c n", n=512)
        self.YF_b = Buf("YF")
        self.S32 = A.alloc(1024 * 4, F32)
        self.S32_b = Buf("S32")
        self.Sbf = A.alloc(1024 * 2)
        self.Sbf_b = Buf("Sbf")
        self.tmp_ring = Ring([(A.alloc(2048, F32), Buf(f"tmp{i}")) for i in range(3)])
        self.Qc_ring = Ring([(A.alloc(512).rearrange("p (a t) -> p a t", t=128), Buf(f"Qc{i}")) for i in range(2)])
        self.Kh_ring = Ring([(A.alloc(512), Buf(f"Kh{i}")) for i in range(2)])
        self.AT_ring = Ring([(A.alloc(256), Buf(f"AT{i}")) for i in range(2)])
        self.qs = A.alloc(8 * 128 * 4, F32).rearrange("p (g n) -> p g n", n=128)
        self.Mp = A.alloc(8 * 128 * 4, F32).rearrange("p (g n) -> p g n", n=128)
        self.rc = A.alloc((4 * 128 + 2) * 4, F32)[:, 0:514]
        self.cols = A.alloc(64 * 4, F32)
        self.dec_b = Buf("dec")
        self.rc_b = Buf("rc")
        self.yst_ring = Ring([(A.alloc(1024).rearrange("p (a t) -> p a t", t=128), Buf(f"yst{i}")) for i in range(2)])
        self.ybf_ring = Ring([(A.alloc(1024), Buf(f"ybf{i}")) for i in range(2)])
        A.pop()

    def ret_decay_setup(self, j):
        Sx = self.S
        c = self.cols
        rc = self.rc
        db = self.dec_b
        dl, ee, lg, nlg, gC = c[:, 0:8], c[:, 8:16], c[:, 16:24], c[:, 24:32], c[:, 32:40]
        kd = c[:, 40:48]
        ksc = c[:, 48:56]
        src = self.rdl[j:j + 1, :].partition_broadcast(128)
        Sx.dma("sp", lambda e: e.dma_start(out=dl, in_=src), writes=[db])
        Sx.op("act", lambda e: e.activation(out=ee, in_=dl, func=AF.Exp), reads=[db], writes=[db])
        Sx.op("act", lambda e: e.activation(out=lg, in_=ee, func=AF.Ln, scale=-1.0, bias=1.0), reads=[db], writes=[db])
        Sx.op("act", lambda e: e.activation(out=nlg, in_=lg, func=AF.Copy, scale=-1.0), reads=[db], writes=[db])
        Sx.op("act", lambda e: e.activation(out=gC, in_=lg, func=AF.Exp, scale=128.0), reads=[db], writes=[db])
        for d in range(2):
            for h in range(4):
                i = d * 4 + h
                ramp = rc[:, 256 + d * 128: 256 + (d + 1) * 128]
                colr = rc[:, 512 + d: 513 + d]
                mask = rc[:, d * 128:(d + 1) * 128]
                Sx.op("act", lambda e, i=i, ramp=ramp: e.activation(out=self.qs[:, i, :], in_=ramp, func=AF.Exp,
                                                                    scale=lg[:, i:i + 1]),
                      reads=[db, self.rc_b], writes=[db])
                Sx.op("act", lambda e, i=i, colr=colr: e.activation(out=ksc[:, i:i + 1], in_=colr, func=AF.Exp,
                                                                    scale=nlg[:, i:i + 1]),
                      reads=[db, self.rc_b], writes=[db])
                Sx.op("dve", lambda e, i=i, mask=mask: e.tensor_scalar(out=self.Mp[:, i, :], in0=mask,
                                                                       scalar1=ksc[:, i:i + 1], scalar2=0.0625,
                                                                       op0=ALU.mult, op1=ALU.mult),
                      reads=[db, self.rc_b], writes=[db])
                Sx.op("dve", lambda e, i=i: e.tensor_scalar(out=kd[:, i:i + 1], in0=ksc[:, i:i + 1],
                                                            scalar1=gC[:, i:i + 1], scalar2=0.0625,
                                                            op0=ALU.mult, op1=ALU.mult),
                      reads=[db], writes=[db])

    def ret_run(self, u):
        Sx = self.S
        layer, s, first = u["layer"], u["s"], u["first"]
        j = layer // 2
        if getattr(self, "phase", None) != "ret":
            self.phase_switch("ret")
        gset = 2 * ((layer * 3 + 1) % 2)
        if s == 0:
            self.load_gain(gset, layer, 2)
            self.load_gain(gset + 1, layer, 3)
            self.ret_decay_setup(j)
        for t in range(16):
            self.prenorm_tile(first, s, t, gset)
        hT, ps = self.hT, self.ps
        cs, QrT, KrT, Krtm, Vtm, YF = self.cs, self.QrT, self.KrT, self.Krtm, self.Vtm, self.YF
        S32, Sbf = self.S32, self.Sbf
        S32_3 = S32.rearrange("p (a n) -> p a n", n=512)
        Sbf_3 = Sbf.rearrange("p (a n) -> p a n", n=512)
        c_ = self.cols
        gC, kd = c_[:, 32:40], c_[:, 40:48]
        ident = self.ident
        db = self.dec_b
        nq = 0
        for h in range(4):
            slot_qk, slot_qk_b = self.w_get()
            wqk = slot_qk.rearrange("p (k n) -> p k n", n=512)
            for which, dstT, dst_b in ((0, QrT, self.QrT_b), (1, KrT, self.KrT_b)):
                for tb in range(4):
                    cols = slice(tb * 512, (tb + 1) * 512)

                    def mmq(pe, wqk=wqk, off=which * 256, cols=cols):
                        for dc in range(2):
                            for k in range(8):
                                ins = pe.matmul(ps[:, dc, :], lhsT=wqk[:, k, off + dc * 128: off + (dc + 1) * 128],
                                                rhs=hT[:, k, cols], start=(k == 0), stop=(k == 7))
                        return ins
                    Sx.op("pe", mmq, reads=[slot_qk_b] + self.hT_b[tb * 4:tb * 4 + 4], writes=[self.bank[0], self.bank[1]])
                    ta, ta_b = self.tmp_ring.next()
                    tb_, tb_b = self.tmp_ring.next()
                    t1p, t2p = ps[:, 0, :], ps[:, 1, :]
                    cosb, sinb = cs[:, 0, cols], cs[:, 1, cols]
                    rd = [self.bank[0], self.bank[1], self.cs_b]
                    Sx.op("dve", lambda e, ta=ta, t1p=t1p, cosb=cosb: e.tensor_tensor(out=ta, in0=t1p, in1=cosb, op=ALU.mult),
                          reads=rd, writes=[ta_b])
                    Sx.op("dve", lambda e, tb_=tb_, t2p=t2p, sinb=sinb: e.tensor_tensor(out=tb_, in0=t2p, in1=sinb, op=ALU.mult),
                          reads=rd, writes=[tb_b])
                    Sx.op("dve", lambda e, ta=ta, tb_=tb_, dstT=dstT, cols=cols: e.tensor_tensor(
                        out=dstT[:, 0, cols], in0=ta, in1=tb_, op=ALU.subtract), reads=[ta_b, tb_b], writes=[dst_b])
                    Sx.op("dve", lambda e, ta=ta, t1p=t1p, sinb=sinb: e.tensor_tensor(out=ta, in0=t1p, in1=sinb, op=ALU.mult),
                          reads=rd, writes=[ta_b])
                    Sx.op("dve", lambda e, tb_=tb_, t2p=t2p, cosb=cosb: e.tensor_tensor(out=tb_, in0=t2p, in1=cosb, op=ALU.mult),
                          reads=rd, writes=[tb_b])
                    Sx.op("dve", lambda e, ta=ta, tb_=tb_, dstT=dstT, cols=cols: e.tensor_tensor(
                        out=dstT[:, 1, cols], in0=ta, in1=tb_, op=ALU.add), reads=[ta_b, tb_b], writes=[dst_b])
            pK = ps[:, 7, 0:512].bitcast(BF16).rearrange("p (c n) -> p c n", n=256)
            for c4 in range(4):
                def trk(pe, c4=c4):
                    for cl in range(4):
                        c = c4 * 4 + cl
                        for dc in range(2):
                            ins = pe.transpose(pK[:, cl, dc * 128:(dc + 1) * 128], KrT[:, dc, c * 128:(c + 1) * 128], ident)
                    return ins
                Sx.op("pe", trk, reads=[self.KrT_b, self.ident_b], writes=[self.bank[7]])
                Sx.op("act", lambda e, c4=c4: e.activation(out=Krtm[:, c4 * 4:c4 * 4 + 4, :], in_=pK, func=AF.Copy),
                      reads=[self.bank[7]], writes=[self.Krtm_b])
            slot_v, slot_v_b = self.w_get()
            wv = slot_v.rearrange("p (k n) -> p k n", n=512)
            for c in range(16):
                bank = 2 + c % 2

                def mmv(pe, wv=wv, c=c, bank=bank):
                    for k in range(8):
                        ins = pe.matmul(ps[:, bank, :], lhsT=hT[:, k, c * 128:(c + 1) * 128], rhs=wv[:, k, :],
                                        start=(k == 0), stop=(k == 7))
                    return ins
                Sx.op("pe", mmv, reads=[slot_v_b, self.hT_b[c]], writes=[self.bank[bank]])
                Sx.op("act", lambda e, c=c, bank=bank: e.activation(out=Vtm[:, c, :], in_=ps[:, bank, :], func=AF.Copy),
                      reads=[self.bank[bank]], writes=[self.Vtm_b])
            for d in range(2):
                i8 = d * 4 + h
                slot_g, slot_g_b = self.w_get()
                wgt = slot_g.rearrange("p (k n) -> p k n", n=512)
                Sx.op("dve", lambda e: e.memset(S32, 0.0), writes=[self.S32_b])
                Sx.op("dve", lambda e: e.memset(Sbf, 0.0), writes=[self.Sbf_b])
                order = range(16) if d == 0 else range(15, -1, -1)
                for c in order:
                    tok = slice(c * 128, (c + 1) * 128)
                    Qc, Qc_b = self.Qc_ring.next()
                    for dc in range(2):
                        Sx.op("dve", lambda e, Qc=Qc, dc=dc, tok=tok, i8=i8: e.tensor_tensor(
                            out=Qc[:, dc, :], in0=QrT[:, dc, tok], in1=self.qs[:, i8, :], op=ALU.mult),
                            reads=[self.QrT_b, db], writes=[Qc_b])
                    ab = 0

                    def mma(pe, Qc=Qc, tok=tok):
                        for dc in range(2):
                            ins = pe.matmul(ps[:, 0, 0:128], lhsT=KrT[:, dc, tok], rhs=Qc[:, dc, :],
                                            start=(dc == 0), stop=(dc == 1))
                        return ins
                    Sx.op("pe", mma, reads=[self.KrT_b, Qc_b], writes=[self.bank[0]])
                    AT, AT_b = self.AT_ring.next()
                    Sx.op("dve", lambda e, AT=AT, i8=i8: e.tensor_tensor(out=AT, in0=ps[:, 0, 0:128], in1=self.Mp[:, i8, :],
                                                                        op=ALU.mult),
                          reads=[self.bank[0], db], writes=[AT_b])
                    yb_ = 1 if nq % 2 == 0 else 6
                    gb_ = 2 + nq % 2
                    nq += 1

                    def mmy(pe, AT=AT, Qc=Qc, c=c, yb_=yb_):
                        pe.matmul(ps[:, yb_, :], lhsT=AT, rhs=Vtm[:, c, :], start=True, stop=False)
                        for dc in range(2):
                            ins = pe.matmul(ps[:, yb_, :], lhsT=Qc[:, dc, :], rhs=Sbf_3[:, dc, :], start=False, stop=(dc == 1))
                        return ins
                    Sx.op("pe", mmy, reads=[AT_b, Qc_b, self.Vtm_b, self.Sbf_b], writes=[self.bank[yb_]])

                    def mmg_(pe, wgt=wgt, tok=tok, gb_=gb_):
                        for k in range(8):
                            ins = pe.matmul(ps[:, gb_, :], lhsT=hT[:, k, tok], rhs=wgt[:, k, :], start=(k == 0), stop=(k == 7))
                        return ins
                    Sx.op("pe", mmg_, reads=[slot_g_b, self.hT_b[c]], writes=[self.bank[gb_]])
                    Kh, Kh_b = self.Kh_ring.next()
                    Sx.op("dve", lambda e, Kh=Kh, c=c, i8=i8: e.tensor_scalar(out=Kh, in0=Krtm[:, c, :], scalar1=kd[:, i8:i8 + 1],
                                                                              scalar2=None, op0=ALU.mult),
                          reads=[self.Krtm_b, db], writes=[Kh_b])

                    def mms(pe, Kh=Kh, c=c):
                        for dc in range(2):
                            ins = pe.matmul(ps[:, 4 + dc, :], lhsT=Kh[:, dc * 128:(dc + 1) * 128], rhs=Vtm[:, c, :],
                                            start=True, stop=True)
                        return ins
                    Sx.op("pe", mms, reads=[Kh_b, self.Vtm_b], writes=[self.bank[4], self.bank[5]])
                    Sx.op("dve", lambda e, i8=i8: e.scalar_tensor_tensor(out=S32_3, in0=S32_3, scalar=gC[:, i8:i8 + 1],
                                                                         in1=ps[:, 4:6, :], op0=ALU.mult, op1=ALU.add),
                          reads=[self.bank[4], self.bank[5], self.S32_b, db], writes=[self.S32_b])
                    Sx.op("act", lambda e: e.activation(out=Sbf, in_=S32, func=AF.Copy), reads=[self.S32_b], writes=[self.Sbf_b])
                    eg, eg_b = self.tmp_ring.next()
                    Sx.op("act", lambda e, eg=eg, gb_=gb_: e.activation(out=eg, in_=ps[:, gb_, :], func=AF.Exp, scale=-1.0),
                          reads=[self.bank[gb_]], writes=[eg_b])
                    Sx.op("dve", lambda e, eg=eg: e.tensor_scalar(out=eg, in0=eg, scalar1=1.0, scalar2=None, op0=ALU.add),
                          reads=[eg_b], writes=[eg_b])
                    Sx.op("dve", lambda e, eg=eg: e.reciprocal(out=eg, in_=eg), reads=[eg_b], writes=[eg_b])
                    Sx.op("dve", lambda e, eg=eg, gb_=gb_: e.tensor_tensor(out=eg, in0=eg, in1=ps[:, gb_, :], op=ALU.mult),
                          reads=[eg_b, self.bank[gb_]], writes=[eg_b])
                    st, st_b = self.stat_ring.next()
                    st2, st2_b = self.stat_ring.next()
                    yn, yn_b = self.tmp_ring.next()
                    ypsum = ps[:, yb_, :]
                    Sx.op("act", lambda e, yn=yn, st=st, ypsum=ypsum: e.activation(out=yn, in_=ypsum, func=AF.Copy,
                                                                                   accum_out=st[:, 0:1]),
                          reads=[self.bank[yb_]], writes=[yn_b, st_b])
                    Sx.op("act", lambda e, yn=yn, st=st, ypsum=ypsum: e.activation(out=yn, in_=ypsum, func=AF.Square,
                                                                                   accum_out=st[:, 1:2]),
                          reads=[self.bank[yb_]], writes=[yn_b, st_b])
                    Sx.op("dve", lambda e, st=st, st2=st2: e.tensor_scalar(out=st2[:, 0:1], in0=st[:, 0:1], scalar1=1.0 / 512,
                                                                           scalar2=None, op0=ALU.mult),
                          reads=[st_b], writes=[st2_b])
                    Sx.op("dve", lambda e, st2=st2: e.tensor_tensor(out=st2[:, 1:2], in0=st2[:, 0:1], in1=st2[:, 0:1], op=ALU.mult),
                          reads=[st2_b], writes=[st2_b])
                    Sx.op("dve", lambda e, st=st, st2=st2: e.scalar_tensor_tensor(out=st2[:, 2:3], in0=st[:, 1:2], scalar=1.0 / 512,
                                                                                  in1=st2[:, 1:2], op0=ALU.mult, op1=ALU.subtract),
                          reads=[st_b, st2_b], writes=[st2_b])
                    Sx.op("act", lambda e, st=st, st2=st2: e.activation(out=st[:, 2:3], in_=st2[:, 2:3], func=AF.Ln, bias=EPS),
                          reads=[st2_b], writes=[st_b])
                    Sx.op("act", lambda e, st=st: e.activation(out=st[:, 3:4], in_=st[:, 2:3], func=AF.Exp, scale=-0.5),
                          reads=[st_b], writes=[st_b])
                    Sx.op("dve", lambda e, yn=yn, st=st, st2=st2, ypsum=ypsum: e.tensor_scalar(
                        out=yn, in0=ypsum, scalar1=st2[:, 0:1], scalar2=st[:, 3:4], op0=ALU.subtract, op1=ALU.mult),
                        reads=[self.bank[yb_], st_b, st2_b], writes=[yn_b])
                    if d == 0:
                        Sx.op("dve", lambda e, yn=yn, eg=eg, c=c: e.tensor_tensor(out=YF[:, c, :], in0=yn, in1=eg, op=ALU.mult),
                              reads=[yn_b, eg_b], writes=[self.YF_b])
                    else:
                        Sx.op("dve", lambda e, yn=yn, eg=eg: e.tensor_tensor(out=yn, in0=yn, in1=eg, op=ALU.mult),
                              reads=[yn_b, eg_b], writes=[yn_b])
                        ybf, ybf_b = self.ybf_ring.next()
                        Sx.op("dve", lambda e, yn=yn, ybf=ybf, c=c: e.tensor_tensor(out=ybf, in0=yn, in1=YF[:, c, :], op=ALU.add),
                              reads=[yn_b, self.YF_b], writes=[ybf_b])
                        pY = ps[:, 7, 0:256].bitcast(BF16).rearrange("p (a t) -> p a t", t=128)

                        def try_(pe, ybf=ybf):
                            for ec in range(4):
                                ins = pe.transpose(pY[:, ec, :], ybf[:, ec * 128:(ec + 1) * 128], ident)
                            return ins
                        Sx.op("pe", try_, reads=[ybf_b, self.ident_b], writes=[self.bank[7]])
                        yst, yst_b = self.yst_ring.next()
                        Sx.op("act", lambda e, yst=yst: e.activation(out=yst, in_=pY, func=AF.Copy),
                              reads=[self.bank[7]], writes=[yst_b])
                        dstd = self.yT_d[s, :, 4 * h:4 * h + 4, c * 128:(c + 1) * 128]
                        Sx.dma("sp", lambda e, yst=yst, dstd=dstd: e.dma_start(out=dstd, in_=yst),
                               reads=[yst_b], writes=[self.yTd_b[s][c // 4]])
        wsl = [self.w_get(hold=i) for i in range(4)]
        yTl = Vtm
        for tb in range(4):
            srcd = self.yT_d[s, :, :, tb * 512:(tb + 1) * 512]
            Sx.dma("sp", lambda e, srcd=srcd: e.dma_start(out=yTl, in_=srcd), reads=[self.yTd_b[s][tb]], writes=[self.Vtm_b])
            for tt in range(4):
                t = tb * 4 + tt
                b0 = 2 * (t % 2)

                def mmo(pe, tt=tt, b0=b0):
                    for dh in range(2):
                        for kc in range(16):
                            w3 = wsl[dh * 2 + kc // 8][0].rearrange("p (k n) -> p k n", n=512)
                            ins = pe.matmul(ps[:, b0 + dh, :], lhsT=yTl[:, kc, tt * 128:(tt + 1) * 128], rhs=w3[:, kc % 8, :],
                                            start=(kc == 0), stop=(kc == 15))
                    return ins
                Sx.op("pe", mmo, reads=[w_[1] for w_ in wsl] + [self.Vtm_b], writes=[self.bank[b0], self.bank[b0 + 1]])
                self.postnorm_tile(first, s, t, b0, gset + 1, 1.0)

    def phase_switch(self, name):
        fence = {}
        for e in ("pe", "act", "dve", "pool"):
            if self.S.tick[e] > 0:
                fence[e] = self.S.tick[e]
        for q in ("sp", "pool", "act"):
            for i in range(DMA_K):
                if self.S.pool_target[q][i] > 0:
                    fence[("dma", q, i)] = self.S.pool_target[q][i]
        self.phase = name
        if name == "ffn":
            self.ffn_alloc()
            locs = self.aT_b + [self.Wd_b] + [b for _, b in self.sg_ring.items]
        elif name == "attn":
            self.attn_alloc()
            locs = ([self.oT_b, self.qT_b, self.kT_b, self.E_b] + self.acc_b + self.den_b + self.Vaug_b
                    + [b for _, b in self.pe32_ring.items] + [b for _, b in self.PT_ring.items])
        elif name == "ret":
            self.ret_alloc()
            locs = ([self.cs_b, self.QrT_b, self.KrT_b, self.Krtm_b, self.Vtm_b, self.YF_b, self.S32_b, self.Sbf_b,
                     self.dec_b, self.rc_b]
                    + [b for rg in (self.tmp_ring, self.Qc_ring, self.Kh_ring, self.AT_ring, self.yst_ring, self.ybf_ring)
                       for _, b in rg.items])
        for bf in locs:
            bf.r = dict(fence)
        if name == "attn":
            self.attn_setup()
        if name == "ret":
            self.S.dma("sp", lambda e: e.dma_start(out=self.cs, in_=self.cs_d), writes=[self.cs_b])
            self.S.dma("sp", lambda e: e.dma_start(out=self.rc, in_=self.rconst), writes=[self.rc_b])


FULL_PLAN = [(l, sub) for l in range(NL) for sub in range(3)]


def t5_buckets(rel):
    half = 16
    max_exact = 8
    n = np.abs(rel)
    large = max_exact + (np.log(np.maximum(n, 1) / max_exact) / np.log(1024 / max_exact) * (half - max_exact)).astype(np.int64)
    large = np.minimum(large, half - 1)
    return ((rel > 0) * half + np.where(n < max_exact, n, large)).astype(np.int32)


def expand_bias(rel_bias):
    kp = np.arange(128)[:, None]
    qi = np.arange(256)[None, :]
    off = kp - qi + 64
    valid = np.abs(off) <= 64
    out = np.full((128, 48, 256), -30000.0, dtype=np.float32)
    for g, dil in enumerate((1, 4, 16)):
        bk = t5_buckets(np.clip(off, -64, 64) * dil)
        for h in range(16):
            tile = rel_bias[g * 16 + h][bk]
            out[:, g * 16 + h, :] = np.where(valid, tile, np.float32(-30000.0))
    return out


def ret_consts():
    jj = np.arange(128)[:, None]
    ii = np.arange(128)[None, :]
    maskf = (ii >= jj).astype(np.float32)
    maskb = (jj >= ii).astype(np.float32)
    rampf = np.broadcast_to((ii + 1).astype(np.float32), (128, 128))
    rampb = np.broadcast_to((128 - ii).astype(np.float32), (128, 128))
    colr = np.concatenate([(jj + 1), (128 - jj)], axis=1).astype(np.float32)
    rconst = np.ascontiguousarray(np.concatenate([maskf, maskb, rampf, rampb, colr], axis=1), dtype=np.float32)
    inv_freq = (1.0 / (np.float32(10000.0) ** np.linspace(0.0, 1.0, 128, dtype=np.float32))).astype(np.float32)
    ang = (np.arange(S, dtype=np.float32)[None, :] * inv_freq[:, None]).astype(np.float32)
    cossin = np.stack([np.cos(ang), np.sin(ang)], axis=1).astype(np.float32)
    return rconst, np.ascontiguousarray(cossin)


def run_plan(inputs, plan, nseq, ncores, trace=False, debug_out=None):
    prog = Prog(nseq, plan, debug_out)
    nc = prog.build()
    x = np.ascontiguousarray(inputs["x"], dtype=np.float32)
    ident = np.eye(128, dtype=np.float32)
    biasexp = expand_bias(np.asarray(inputs["rel_bias"], dtype=np.float32))
    rconst, cossin = ret_consts()
    in_maps = []
    for c in range(ncores):
        m = {"x": x[c * nseq:(c + 1) * nseq].reshape(nseq * S, D),
             "norm_gains": np.ascontiguousarray(inputs["norm_gains"], dtype=np.float32),
             "ffn_w_gate": np.ascontiguousarray(inputs["ffn_w_gate"], dtype=np.float32),
             "ffn_w_up": np.ascontiguousarray(inputs["ffn_w_up"], dtype=np.float32),
             "ffn_w_down": np.ascontiguousarray(inputs["ffn_w_down"], dtype=np.float32),
             "ident": ident,
             "attn_w_in": np.ascontiguousarray(inputs["attn_w_in"], dtype=np.float32),
             "attn_w_out": np.ascontiguousarray(inputs["attn_w_out"], dtype=np.float32),
             "biasexp": biasexp,
             "ret_w_in": np.ascontiguousarray(inputs["ret_w_in"], dtype=np.float32),
             "ret_w_out": np.ascontiguousarray(inputs["ret_w_out"], dtype=np.float32),
             "ret_decay_logit": np.ascontiguousarray(inputs["ret_decay_logit"], dtype=np.float32).reshape(2, 8),
             "rconst": rconst, "cossin": cossin}
        in_maps.append(m)
    res = run_bass_kernel_spmd(nc, in_maps, core_ids=list(range(ncores)), trace=trace)
    out = np.stack([r["y"].reshape(nseq, S, D) for r in res.results], axis=0).reshape(ncores * nseq, S, D)
    if debug_out is not None:
        return out, res, {k: res.results[0][k] for k in prog.dbg_names}
    return out, res


def kernel(**inputs):
    out, _ = run_plan(inputs, FULL_PLAN, 2, NCORES)
    return out.astype(np.float32)
```

```python
import contextlib
import numpy as np
import concourse.bass as bass
import concourse.mybir as mybir
from concourse.bass_utils import run_bass_kernel_spmd

F32 = mybir.dt.float32
BF16 = mybir.dt.bfloat16
AF = mybir.ActivationFunctionType
ALU = mybir.AluOpType

D = 1024
S = 2048
DFF = 2816
NCH = DFF // 128
NL = 4
EPS = 1e-6
NCORES = 8
NSLOT = 4
DMA_K = 6


class Buf:
    __slots__ = ("name", "w", "r")

    def __init__(self, name):
        self.name = name
        self.w = None
        self.r = {}


class Sched:
    ENG = ("pe", "act", "dve", "pool", "sp")

    def __init__(self):
        self.streams = {e: [] for e in self.ENG}
        self.tick = {e: 0 for e in self.ENG}
        self.waited = {e: {} for e in self.ENG}
        self.pool_next = {q: 0 for q in ("sp", "pool", "act")}
        self.pool_target = {q: [0] * DMA_K for q in ("sp", "pool", "act")}

    def _waits(self, eng, reads, writes):
        need = {}

        def add(sem, val):
            if need.get(sem, 0) < val:
                need[sem] = val

        for b in reads:
            if b.w is not None:
                add(*b.w)
        for b in writes:
            if b.w is not None:
                add(*b.w)
            for sem, val in b.r.items():
                add(sem, val)
        out = []
        wd = self.waited[eng]
        for sem, val in need.items():
            if sem == "pe" and eng == "pe":
                continue
            if wd.get(sem, 0) >= val:
                continue
            wd[sem] = val
            out.append((sem, val))
        return out

    @staticmethod
    def _update(ev, reads, writes):
        sem, val = ev
        for b in reads:
            if b.r.get(sem, 0) < val:
                b.r[sem] = val
        for b in writes:
            b.w = ev
            b.r = {}

    def op(self, eng, fn, reads=(), writes=()):
        waits = self._waits(eng, reads, writes)
        self.tick[eng] += 1
        ev = (eng, self.tick[eng])
        self.streams[eng].append((waits, fn, ev, 1))
        self._update(ev, reads, writes)
        return ev

    def dma(self, q, fn, reads=(), writes=()):
        i = self.pool_next[q]
        self.pool_next[q] = (i + 1) % DMA_K
        semkey = ("dma", q, i)
        waits = self._waits(q, reads, writes)
        prev = self.pool_target[q][i]
        if prev > 0 and self.waited[q].get(semkey, 0) < prev:
            self.waited[q][semkey] = prev
            waits.append((semkey, prev))
        self.pool_target[q][i] = prev + 16
        ev = (semkey, prev + 16)
        self.streams[q].append((waits, fn, ev, 16))
        self._update(ev, reads, writes)
        return ev

    def sem_keys(self):
        keys = [e for e in self.ENG if e != "sp"]
        for q in ("sp", "pool", "act"):
            for i in range(DMA_K):
                keys.append(("dma", q, i))
        return keys

    def emit(self, nc, block, semh):
        final = []
        for q in ("sp", "pool", "act"):
            for i in range(DMA_K):
                if self.pool_target[q][i] > 0:
                    final.append((("dma", q, i), self.pool_target[q][i]))
        for e in ("pe", "act", "dve", "pool"):
            if self.tick[e] > 0:
                final.append((e, self.tick[e]))

        def run(eng, stream, tail=False):
            for waits, fn, ev, inc in stream:
                for sem, val in waits:
                    eng.wait_ge(semh[sem], val)
                ins = fn(eng)
                ins.then_inc(semh[ev[0]], inc)
            if tail:
                for sem, val in final:
                    eng.wait_ge(semh[sem], val)

        @block.tensor
        def _(pe):
            run(pe, self.streams["pe"])

        @block.scalar
        def _(act):
            run(act, self.streams["act"])

        @block.vector
        def _(dve):
            run(dve, self.streams["dve"])

        @block.gpsimd
        def _(pool):
            run(pool, self.streams["pool"])

        @block.sync
        def _(sp):
            run(sp, self.streams["sp"], tail=True)


class Ring:
    def __init__(self, items):
        self.items = items
        self.i = 0

    def next(self):
        it = self.items[self.i]
        self.i = (self.i + 1) % len(self.items)
        return it


class Arena:
    def __init__(self, ap, nbytes):
        self.ap = ap
        self.nbytes = nbytes
        self.off = 0
        self.marks = []

    def alloc(self, nbytes, dtype=BF16):
        nbytes = (nbytes + 63) // 64 * 64
        assert self.off + nbytes <= self.nbytes, ("SBUF arena overflow", self.off, nbytes, self.nbytes)
        a = self.ap[:, self.off // 2:(self.off + nbytes) // 2]
        self.off += nbytes
        if dtype == F32:
            a = a.bitcast(F32)
        return a

    def push(self):
        self.marks.append(self.off)

    def pop(self):
        self.off = self.marks.pop()


class Prog:
    def __init__(self, nseq, plan, debug_out=None):
        self.nseq = nseq
        self.plan = plan
        self.debug_out = debug_out
        self.dbg_names = []
        self.S = Sched()
        self.nc = bass.Bass("TRN2", target_bir_lowering=False)
        nc = self.nc
        T = nseq * S
        self.x_in = nc.dram_tensor("x", [T, D], F32, kind="ExternalInput").ap()
        self.y = nc.dram_tensor("y", [T, D], F32, kind="ExternalOutput").ap()
        self.gains = nc.dram_tensor("norm_gains", [NL, 6, D], F32, kind="ExternalInput").ap()
        self.wg = nc.dram_tensor("ffn_w_gate", [NL, 2, D, DFF], F32, kind="ExternalInput").ap()
        self.wu = nc.dram_tensor("ffn_w_up", [NL, 2, D, DFF], F32, kind="ExternalInput").ap()
        self.wd = nc.dram_tensor("ffn_w_down", [NL, 2, DFF, D], F32, kind="ExternalInput").ap()
        self.ident_d = nc.dram_tensor("ident", [128, 128], F32, kind="ExternalInput").ap()
        self.awin = nc.dram_tensor("attn_w_in", [2, D, 9216], F32, kind="ExternalInput").ap()
        self.awout = nc.dram_tensor("attn_w_out", [2, D, D], F32, kind="ExternalInput").ap()
        self.biasexp = nc.dram_tensor("biasexp", [128, 48, 256], F32, kind="ExternalInput").ap()
        self.rwin = nc.dram_tensor("ret_w_in", [2, D, 8192], F32, kind="ExternalInput").ap()
        self.rwout = nc.dram_tensor("ret_w_out", [2, 2048, D], F32, kind="ExternalInput").ap()
        self.rdl = nc.dram_tensor("ret_decay_logit", [2, 8], F32, kind="ExternalInput").ap()
        self.rconst = nc.dram_tensor("rconst", [128, 4 * 128 + 2], F32, kind="ExternalInput").ap()
        self.cs_d = nc.dram_tensor("cossin", [128, 2, S], F32, kind="ExternalInput").ap()
        self.yT_d = nc.dram_tensor("yT_scratch", [nseq, 128, 16, S], BF16, kind="Internal").ap()

    def debug(self, name, ap, reads):
        if self.debug_out is None or name not in self.debug_out:
            return
        shape = list(ap.shape)
        d = self.nc.dram_tensor("dbg_" + name, shape, F32, kind="ExternalOutput").ap()
        self.S.dma("pool", lambda e: e.dma_start(out=d, in_=ap), reads=reads)
        self.dbg_names.append("dbg_" + name)

    def bufs(self, name, n):
        return [Buf(f"{name}{i}") for i in range(n)]

    def build(self):
        nc = self.nc
        Sx = self.S
        with contextlib.ExitStack() as es:
            ARENA_BYTES = 207 * 1024
            arena_t = es.enter_context(nc.sbuf_tensor("arena", [128, ARENA_BYTES // 2], BF16))
            ps_t = es.enter_context(nc.psum_tensor("ps", [128, 8, 512], F32))
            self.ps = ps_t
            self.bank = self.bufs("bank", 8)
            A = Arena(arena_t, ARENA_BYTES)
            self.A = A
            self.ident = A.alloc(256)
            self.ident_b = Buf("ident")
            self.mhalf = A.alloc(64, F32)
            self.mhalf_b = Buf("mhalf")
            Sx.op("pool", lambda e: e.memset(self.mhalf, -0.5), writes=[self.mhalf_b])
            self.stat = A.alloc(16 * 16, F32)
            self.stat_ring = Ring([(self.stat[:, 4 * i:4 * i + 4], Buf(f"stat{i}")) for i in range(16)])
            self.gain = [(A.alloc(4096, F32), Buf(f"gain{i}")) for i in range(4)]
            self.xt_ring = Ring([(A.alloc(4096, F32), Buf(f"xt{i}")) for i in range(3)])
            self.hb_ring = Ring([(A.alloc(2048), Buf(f"hb{i}")) for i in range(2)])
            self.t1_ring = Ring([(A.alloc(4096, F32), Buf(f"t1_{i}")) for i in range(2)])
            self.hT = A.alloc(8 * S * 2).rearrange("p (k t) -> p k t", t=S)
            self.hT_b = self.bufs("hT", 16)
            self.wring = [(A.alloc(8192), Buf(f"wslot{i}")) for i in range(NSLOT)]
            self.x_b = [self.bufs(f"x{s}_", 16) for s in range(self.nseq)]
            self.yTd_b = [self.bufs(f"yTd{s}_", 4) for s in range(self.nseq)]
            self.x_first = True

            Sx.dma("pool", lambda e: e.dma_start(out=self.ident, in_=self.ident_d), writes=[self.ident_b])

            units = self.make_units()
            self.slabs = []
            for u in units:
                u["slab0"] = len(self.slabs)
                self.slabs.extend(u["slabs"])
            self.slab_issued = 0
            self.slab_next = 0
            for u in units:
                u["run"](u)

            sem_keys = Sx.sem_keys()
            semh = {}
            for i, k in enumerate(sem_keys):
                semh[k] = es.enter_context(nc.semaphore(f"s{i}"))
            block = es.enter_context(nc.Block())
            Sx.emit(nc, block, semh)
        return nc

    def w_issue_upto(self, idx):
        while self.slab_issued <= min(idx, len(self.slabs) - 1):
            i = self.slab_issued
            slot_ap, slot_b = self.wring[i % NSLOT]
            for (dst_fn, src) in self.slabs[i]:
                dst = dst_fn(slot_ap)
                self.S.dma("pool", (lambda d, s_: (lambda e: e.dma_start(out=d, in_=s_)))(dst, src), writes=[slot_b])
            self.slab_issued += 1

    def w_get(self, hold=0):
        i = self.slab_next
        self.slab_next += 1
        self.w_issue_upto(i + NSLOT - 1 - hold)
        return self.wring[i % NSLOT]

    def x_rows(self, first, s, t):
        src = self.x_in if first else self.y
        r0 = s * S + t * 128
        return src[r0:r0 + 128, :]

    def load_gain(self, slot, layer, idx):
        g_ap, g_b = self.gain[slot]
        src = self.gains[layer, idx:idx + 1, :].partition_broadcast(128)
        self.S.dma("sp", lambda e: e.dma_start(out=g_ap, in_=src), writes=[g_b])

    def rstd_ops(self, st, st_b, n):
        Sx = self.S
        Sx.op("dve", lambda e: e.tensor_scalar(out=st[:, 1:2], in0=st[:, 0:1], scalar1=1.0 / n, scalar2=EPS,
                                               op0=ALU.mult, op1=ALU.add),
              reads=[st_b], writes=[st_b])
        Sx.op("pool", lambda e: e.tensor_tensor(out=st[:, 2:3], in0=st[:, 1:2], in1=self.mhalf[:, 0:1], op=ALU.pow),
              reads=[st_b, self.mhalf_b], writes=[st_b])

    def prenorm_tile(self, first, s, t, gslot):
        Sx = self.S
        xs, xs_b = self.xt_ring.next()
        hb, hb_b = self.hb_ring.next()
        st, st_b = self.stat_ring.next()
        g_ap, g_b = self.gain[gslot]
        src = self.x_rows(first, s, t)
        Sx.dma("sp", lambda e: e.dma_start(out=xs, in_=src), reads=[self.x_b[s][t]], writes=[xs_b])
        Sx.op("act", lambda e: e.activation(out=hb, in_=xs, func=AF.Square, accum_out=st[:, 0:1]),
              reads=[xs_b], writes=[hb_b, st_b])
        self.rstd_ops(st, st_b, D)
        Sx.op("dve", lambda e: e.scalar_tensor_tensor(out=hb, in0=xs, scalar=st[:, 2:3], in1=g_ap,
                                                      op0=ALU.mult, op1=ALU.mult),
              reads=[xs_b, st_b, g_b], writes=[hb_b])
        pT = self.ps[:, 7, 0:512].bitcast(BF16).rearrange("p (k t) -> p k t", t=128)
        ident = self.ident

        def tr(pe):
            for k in range(8):
                ins = pe.transpose(pT[:, k, :], hb[:, k * 128:(k + 1) * 128], ident)
            return ins
        Sx.op("pe", tr, reads=[hb_b, self.ident_b], writes=[self.bank[7]])
        hT = self.hT
        Sx.op("act", lambda e: e.activation(out=hT[:, :, t * 128:(t + 1) * 128], in_=pT, func=AF.Copy),
              reads=[self.bank[7]], writes=[self.hT_b[t]])

    def postnorm_tile(self, first, s, t, b0, gslot, coef):
        Sx = self.S
        m = self.ps[:, b0:b0 + 2, :].rearrange("p a b -> p (a b)")
        mb = [self.bank[b0], self.bank[b0 + 1]]
        t1, t1_b = self.t1_ring.next()
        st, st_b = self.stat_ring.next()
        xs, xs_b = self.xt_ring.next()
        g_ap, g_b = self.gain[gslot]
        src = self.x_rows(first, s, t)
        dst = self.y[s * S + t * 128: s * S + t * 128 + 128, :]
        Sx.dma("sp", lambda e: e.dma_start(out=xs, in_=src), reads=[self.x_b[s][t]], writes=[xs_b])
        Sx.op("act", lambda e: e.activation(out=t1, in_=m, func=AF.Square, accum_out=st[:, 0:1]),
              reads=mb, writes=[t1_b, st_b])
        self.rstd_ops(st, st_b, D)
        Sx.op("dve", lambda e: e.scalar_tensor_tensor(out=t1, in0=m, scalar=st[:, 2:3], in1=g_ap,
                                                      op0=ALU.mult, op1=ALU.mult),
              reads=mb + [st_b, g_b], writes=[t1_b])
        Sx.op("dve", lambda e: e.scalar_tensor_tensor(out=xs, in0=t1, scalar=float(coef), in1=xs,
                                                      op0=ALU.mult, op1=ALU.add),
              reads=[t1_b, xs_b], writes=[xs_b])
        Sx.dma("sp", lambda e: e.dma_start(out=dst, in_=xs), reads=[xs_b], writes=[self.x_b[s][t]])

    def make_units(self):
        units = []
        for (layer, sub) in self.plan:
            if sub in (0, 2):
                f = 0 if sub == 0 else 1
                for s in range(self.nseq):
                    for b in range(2):
                        units.append(self.ffn_unit(layer, f, s, b))
            elif layer % 2 == 0:
                for s in range(self.nseq):
                    units.append(self.attn_unit(layer, s))
            else:
                for s in range(self.nseq):
                    units.append(self.ret_unit(layer, s))
        return units

    def ffn_unit(self, layer, f, s, b):
        wg, wu = self.wg[layer, f], self.wu[layer, f]
        slabs = []
        for j in range(NCH // 2):
            c0 = j * 256
            gsrc = wg[:, c0:c0 + 256].rearrange("(k p) n -> p k n", p=128)
            usrc = wu[:, c0:c0 + 256].rearrange("(k p) n -> p k n", p=128)
            slabs.append([
                (lambda sl: sl.rearrange("p (k n) -> p k n", n=512)[:, :, 0:256], gsrc),
                (lambda sl: sl.rearrange("p (k n) -> p k n", n=512)[:, :, 256:512], usrc),
            ])
        first = (layer, 0 if f == 0 else 2) == tuple(self.plan[0])
        return dict(kind="ffn", layer=layer, f=f, s=s, b=b, slabs=slabs, run=self.ffn_run, first=first)

    def ffn_alloc(self):
        A = self.A
        A.push()
        self.aT = A.alloc(NCH * 1024 * 2).rearrange("p (c t) -> p c t", t=1024)
        self.aT_b = self.bufs("aT", 2)
        self.Wd = A.alloc(NCH * 1024 * 2).rearrange("p (c n) -> p c n", n=1024)
        self.Wd_b = Buf("Wd")
        self.sg_ring = Ring([(A.alloc(2048, F32), Buf(f"sg{i}")) for i in range(2)])
        self.Wd_loaded = None
        A.pop()

    def ffn_run(self, u):
        Sx = self.S
        layer, f, s, b = u["layer"], u["f"], u["s"], u["b"]
        first = u["first"]
        if getattr(self, "phase", None) != "ffn":
            self.phase_switch("ffn")
        gset = 2 * ((layer * 3 + 2 * f) % 2)
        if s == 0 and b == 0:
            self.load_gain(gset, layer, 0 if f == 0 else 4)
            self.load_gain(gset + 1, layer, 1 if f == 0 else 5)
        for t in range(8 * b, 8 * b + 8):
            self.prenorm_tile(first, s, t, gset)
        hT, aT, Wd, ps = self.hT, self.aT, self.Wd, self.ps
        n = 0
        for j in range(NCH // 2):
            slot, slot_b = self.w_get()
            if j == 0 and self.Wd_loaded != (layer, f):
                self.Wd_loaded = (layer, f)
                wd = self.wd[layer, f]
                for q in range(2):
                    src = wd[q * 1408:(q + 1) * 1408, :].rearrange("(c p) n -> p c n", p=128)
                    dstw = Wd[:, q * 11:(q + 1) * 11, :]
                    Sx.dma("pool", (lambda d_, s_: (lambda e: e.dma_start(out=d_, in_=s_)))(dstw, src),
                           writes=[self.Wd_b])
            w3 = slot.rearrange("p (k n) -> p k n", n=512)
            for cc in range(2):
                c = 2 * j + cc
                for tb in range(2):
                    col0 = b * 1024 + tb * 512
                    G, U = n % 2, 2 + n % 2
                    n += 1
                    hbufs = self.hT_b[col0 // 128: col0 // 128 + 4]

                    def mmg(pe, off=cc * 128, col0=col0, bank=G, w3=w3):
                        for k in range(8):
                            ins = pe.matmul(ps[:, bank, :], lhsT=w3[:, k, off:off + 128], rhs=hT[:, k, col0:col0 + 512],
                                            start=(k == 0), stop=(k == 7))
                        return ins
                    Sx.op("pe", mmg, reads=[slot_b] + hbufs, writes=[self.bank[G]])

                    def mmu(pe, off=256 + cc * 128, col0=col0, bank=U, w3=w3):
                        for k in range(8):
                            ins = pe.matmul(ps[:, bank, :], lhsT=w3[:, k, off:off + 128], rhs=hT[:, k, col0:col0 + 512],
                                            start=(k == 0), stop=(k == 7))
                        return ins
                    Sx.op("pe", mmu, reads=[slot_b] + hbufs, writes=[self.bank[U]])
                    sg, sg_b = self.sg_ring.next()
                    Sx.op("act", lambda e, sg=sg, bank=G: e.activation(out=sg, in_=ps[:, bank, :], func=AF.Silu),
                          reads=[self.bank[G]], writes=[sg_b])
                    Sx.op("dve", lambda e, sg=sg, bank=U, c=c, tb=tb: e.tensor_tensor(
                        out=aT[:, c, tb * 512:(tb + 1) * 512], in0=sg, in1=ps[:, bank, :], op=ALU.mult),
                        reads=[sg_b, self.bank[U]], writes=[self.aT_b[tb]])
        if b == 0 and s == 0:
            self.debug("hT", hT[:, :, 0:1024], self.hT_b[0:8])
            self.debug("aT", aT, self.aT_b)
            self.debug("Wd", Wd, [self.Wd_b])
        for tt in range(8):
            b0 = 4 + 2 * (tt % 2)

            def mmd(pe, tt=tt, b0=b0):
                for h in range(2):
                    for c in range(NCH):
                        ins = pe.matmul(ps[:, b0 + h, :], lhsT=aT[:, c, tt * 128:(tt + 1) * 128],
                                        rhs=Wd[:, c, h * 512:(h + 1) * 512], start=(c == 0), stop=(c == NCH - 1))
                return ins
            Sx.op("pe", mmd, reads=[self.aT_b[tt // 4], self.Wd_b], writes=[self.bank[b0], self.bank[b0 + 1]])
            self.postnorm_tile(first, s, 8 * b + tt, b0, gset + 1, 0.5)

    def attn_unit(self, layer, s):
        j = layer // 2
        win = self.awin[j]
        slabs = []
        for p in range(8):
            for g in range(3):
                parts = []
                for qi in range(3):
                    c0 = g * 3072 + qi * 1024 + p * 128
                    src = win[:, c0:c0 + 128].rearrange("(k p) n -> p k n", p=128)
                    parts.append(((lambda sl, qi=qi: sl.rearrange("p (k n) -> p k n", n=512)[:, :, qi * 128:(qi + 1) * 128]), src))
                slabs.append(parts)
        wout = self.awout[j]
        for h in range(2):
            src = wout[:, h * 512:(h + 1) * 512].rearrange("(k p) n -> p k n", p=128)
            slabs.append([((lambda sl: sl.rearrange("p (k n) -> p k n", n=512)), src)])
        first = (layer, 1) == tuple(self.plan[0])
        return dict(kind="attn", layer=layer, s=s, slabs=slabs, run=self.attn_run, first=first)

    def attn_alloc(self):
        A = self.A
        A.push()
        self.oT = A.alloc(8 * S * 2).rearrange("p (k t) -> p k t", t=S)
        self.oT_b = Buf("oT")
        self.acc = [A.alloc(S * 4, F32), A.alloc(S * 4, F32)]
        self.acc_b = self.bufs("acc", 2)
        self.den = A.alloc(S * 4, F32)
        self.den_b = self.bufs("den", 2)
        self.qT = A.alloc(S * 2)
        self.kT = A.alloc(S * 2)
        self.qT_b, self.kT_b = Buf("qT"), Buf("kT")
        self.Vaug = [A.alloc(16 * 128 * 2).rearrange("p (c n) -> p c n", n=128) for _ in range(2)]
        self.Vaug_b = self.bufs("Vaug", 2)
        self.E = A.alloc(48 * 256 * 2).rearrange("p (g n) -> p g n", n=256)
        self.E_b = Buf("E")
        self.pe32_ring = Ring([(A.alloc(1024, F32), Buf(f"pe32_{i}")) for i in range(2)])
        self.PT_ring = Ring([(A.alloc(512), Buf(f"PT{i}")) for i in range(2)])
        A.pop()

    def attn_setup(self):
        Sx = self.S
        E = self.E
        for q in range(4):
            stage = self.acc[0] if q % 2 == 0 else self.acc[1]
            stage_b = self.acc_b[q % 2]
            st3 = stage.rearrange("p (g n) -> p g n", n=256)[:, 0:8, :]
            src = self.biasexp[:, q * 12:q * 12 + 8, :]
            Sx.dma("sp", lambda e, st3=st3, src=src: e.dma_start(out=st3, in_=src), writes=[stage_b])
            Sx.op("act", lambda e, st3=st3, q=q: e.activation(out=E[:, q * 12:q * 12 + 8, :], in_=st3, func=AF.Exp),
                  reads=[stage_b], writes=[self.E_b])
            st4 = stage.rearrange("p (g n) -> p g n", n=256)[:, 0:4, :]
            src2 = self.biasexp[:, q * 12 + 8:q * 12 + 12, :]
            Sx.dma("sp", lambda e, st4=st4, src2=src2: e.dma_start(out=st4, in_=src2), reads=[], writes=[stage_b])
            Sx.op("act", lambda e, st4=st4, q=q: e.activation(out=E[:, q * 12 + 8:q * 12 + 12, :], in_=st4, func=AF.Exp),
                  reads=[stage_b], writes=[self.E_b])
        for i in range(2):
            Sx.op("dve", lambda e, i=i: e.memset(self.Vaug[i], 1.0), writes=[self.Vaug_b[i]])

    def attn_run(self, u):
        Sx = self.S
        layer, s, first = u["layer"], u["s"], u["first"]
        if getattr(self, "phase", None) != "attn":
            self.phase_switch("attn")
        gset = 2 * ((layer * 3 + 1) % 2)
        if s == 0:
            self.load_gain(gset, layer, 2)
            self.load_gain(gset + 1, layer, 3)
        for t in range(16):
            self.prenorm_tile(first, s, t, gset)
        hT, ps, oT = self.hT, self.ps, self.oT
        qT, kT = self.qT, self.kT
        nproj = 0
        nv = 0
        nst = 0
        for p in range(8):
            for i in range(2):
                Sx.op("dve", lambda e, i=i: e.memset(self.acc[i], 0.0), writes=[self.acc_b[i]])
            for g in range(3):
                r = (1, 4, 16)[g]
                Ls = S // r
                nb = Ls // 128
                slot, slot_b = self.w_get()
                w3 = slot.rearrange("p (k n) -> p k n", n=512)
                for which, dstT, dst_b, scale in ((0, qT, self.qT_b, 0.125), (1, kT, self.kT_b, 1.0)):
                    for tb in range(4):
                        bank = nproj % 2
                        nproj += 1

                        def mmp(pe, w3=w3, off=which * 128, tb=tb, bank=bank):
                            for k in range(8):
                                ins = pe.matmul(ps[:, bank, :], lhsT=w3[:, k, off:off + 128],
                                                rhs=hT[:, k, tb * 512:(tb + 1) * 512], start=(k == 0), stop=(k == 7))
                            return ins
                        Sx.op("pe", mmp, reads=[slot_b] + self.hT_b[tb * 4:tb * 4 + 4], writes=[self.bank[bank]])
                        Sx.op("act", lambda e, dstT=dstT, tb=tb, bank=bank, scale=scale: e.activation(
                            out=dstT[:, tb * 512:(tb + 1) * 512], in_=ps[:, bank, :], func=AF.Copy, scale=scale),
                            reads=[self.bank[bank]], writes=[dst_b])
                hT4 = hT.rearrange("p k (t r) -> p k t r", r=r)
                for c4 in range(4):
                    bank = 2 + nv % 2
                    nv += 1
                    pv3 = ps[:, bank, :].rearrange("p (c n) -> p c n", n=128)

                    def mmv(pe, w3=w3, c4=c4, pv3=pv3, hT4=hT4, nb=nb):
                        for cl in range(4):
                            ci = c4 * 4 + cl
                            res, jb = ci // nb, ci % nb
                            for k in range(8):
                                ins = pe.matmul(pv3[:, cl, :], lhsT=hT4[:, k, jb * 128:(jb + 1) * 128, res],
                                                rhs=w3[:, k, 256:384], start=(k == 0), stop=(k == 7))
                        return ins
                    Sx.op("pe", mmv, reads=[slot_b] + self.hT_b, writes=[self.bank[bank]])
                    Sx.op("act", lambda e, c4=c4, pv3=pv3: e.activation(
                        out=self.Vaug[0][:, c4 * 4:c4 * 4 + 4, 0:64], in_=pv3[:, :, 0:64], func=AF.Copy),
                        reads=[self.bank[bank]], writes=[self.Vaug_b[0]])
                    Sx.op("act", lambda e, c4=c4, pv3=pv3: e.activation(
                        out=self.Vaug[1][:, c4 * 4:c4 * 4 + 4, 64:128], in_=pv3[:, :, 64:128], func=AF.Copy),
                        reads=[self.bank[bank]], writes=[self.Vaug_b[1]])
                q3 = qT.rearrange("p (t r) -> p t r", r=r)
                k3 = kT.rearrange("p (t r) -> p t r", r=r)
                work = [(e_, ci) for e_ in range(2) for ci in range(16)]
                pend = None

                def st_stage(e_, ci):
                    nonlocal nst
                    res, jb = ci // nb, ci % nb
                    qlo, qhi = max(0, 128 * jb - 64), min(Ls, 128 * jb + 192)
                    nq = qhi - qlo
                    qi0 = qlo - (128 * jb - 64)
                    bank = 4 + nst % 2
                    nst += 1
                    hp = slice(64 * e_, 64 * e_ + 64)
                    l_ap = k3[hp, jb * 128:(jb + 1) * 128, res]
                    r_ap = q3[hp, qlo:qhi, res]
                    Sx.op("pe", lambda pe, bank=bank: pe.matmul(
                        ps[:, bank, 0:nq], lhsT=l_ap, rhs=r_ap, start=True, stop=True),
                        reads=[self.qT_b, self.kT_b], writes=[self.bank[bank]])
                    pe32, pe32_b = self.pe32_ring.next()
                    PT, PT_b = self.PT_ring.next()
                    Sx.op("act", lambda e, bank=bank: e.activation(out=pe32[:, 0:nq], in_=ps[:, bank, 0:nq], func=AF.Exp),
                          reads=[self.bank[bank]], writes=[pe32_b])
                    gh = g * 16 + 2 * p + e_
                    e_ap = self.E[:, gh, qi0:qi0 + nq]
                    Sx.op("dve", lambda e: e.tensor_tensor(out=PT[:, 0:nq], in0=pe32[:, 0:nq], in1=e_ap, op=ALU.mult),
                          reads=[pe32_b, self.E_b], writes=[PT_b])
                    return (e_, ci, res, qlo, qhi, nq, PT, PT_b, bank)

                def pv_stage(stg):
                    e_, ci, res, qlo, qhi, nq, PT, PT_b, bank = stg
                    ob = bank + 2
                    v_ap = self.Vaug[e_][:, ci, :]
                    Sx.op("pe", lambda pe: pe.matmul(ps[:, ob, 0:nq], lhsT=v_ap, rhs=PT[:, 0:nq], start=True, stop=True),
                          reads=[self.Vaug_b[e_], PT_b], writes=[self.bank[ob]])
                    a3 = self.acc[e_].rearrange("p (t r) -> p t r", r=r)
                    Sx.op("dve", lambda e: e.tensor_tensor(out=a3[:, qlo:qhi, res], in0=a3[:, qlo:qhi, res],
                                                           in1=ps[:, ob, 0:nq], op=ALU.add),
                          reads=[self.bank[ob], self.acc_b[e_]], writes=[self.acc_b[e_]])

                for (e_, ci) in work:
                    stg = st_stage(e_, ci)
                    if pend is not None:
                        pv_stage(pend)
                    pend = stg
                pv_stage(pend)
            den = self.den
            Sx.dma("sp", lambda e: e.dma_start(out=den[0:64, :], in_=self.acc[0][64:128, :]),
                   reads=[self.acc_b[0]], writes=[self.den_b[0]])
            Sx.dma("sp", lambda e: e.dma_start(out=den[64:128, :], in_=self.acc[1][0:64, :]),
                   reads=[self.acc_b[1]], writes=[self.den_b[1]])
            Sx.op("dve", lambda e: e.reciprocal(out=den, in_=den), reads=self.den_b, writes=self.den_b)
            Sx.op("dve", lambda e, p=p: e.tensor_tensor(out=oT[0:64, p, :], in0=self.acc[0][0:64, :], in1=den[0:64, :],
                                                        op=ALU.mult),
                  reads=[self.acc_b[0]] + self.den_b, writes=[self.oT_b])
            Sx.op("dve", lambda e, p=p: e.tensor_tensor(out=oT[64:128, p, :], in0=self.acc[1][64:128, :],
                                                        in1=den[64:128, :], op=ALU.mult),
                  reads=[self.acc_b[1]] + self.den_b, writes=[self.oT_b])
        slots = [self.w_get(), self.w_get(hold=1)]
        for t in range(16):
            b0 = 2 * (t % 2)

            def mmo(pe, t=t, b0=b0):
                for h in range(2):
                    w3 = slots[h][0].rearrange("p (k n) -> p k n", n=512)
                    for k in range(8):
                        ins = pe.matmul(ps[:, b0 + h, :], lhsT=oT[:, k, t * 128:(t + 1) * 128], rhs=w3[:, k, :],
                                        start=(k == 0), stop=(k == 7))
                return ins
            Sx.op("pe", mmo, reads=[slots[0][1], slots[1][1], self.oT_b], writes=[self.bank[b0], self.bank[b0 + 1]])
            self.postnorm_tile(first, s, t, b0, gset + 1, 1.0)

    def ret_unit(self, layer, s):
        j = layer // 2
        win = self.rwin[j]
        full = lambda sl: sl.rearrange("p (k n) -> p k n", n=512)
        slabs = []
        for h in range(4):
            qsrc = win[:, h * 256:(h + 1) * 256].rearrange("(k p) n -> p k n", p=128)
            ksrc = win[:, 1024 + h * 256:1024 + (h + 1) * 256].rearrange("(k p) n -> p k n", p=128)
            slabs.append([((lambda sl: sl.rearrange("p (k n) -> p k n", n=512)[:, :, 0:256]), qsrc),
                          ((lambda sl: sl.rearrange("p (k n) -> p k n", n=512)[:, :, 256:512]), ksrc)])
            for base in (2048, 4096, 6144):
                src = win[:, base + h * 512: base + (h + 1) * 512].rearrange("(k p) n -> p k n", p=128)
                slabs.append([(full, src)])
        wout = self.rwout[j]
        for dh in range(2):
            for kh in range(2):
                src = wout[kh * 1024:(kh + 1) * 1024, dh * 512:(dh + 1) * 512].rearrange("(k p) n -> p k n", p=128)
                slabs.append([(full, src)])
        first = (layer, 1) == tuple(self.plan[0])
        return dict(kind="ret", layer=layer, s=s, slabs=slabs, run=self.ret_run, first=first)

    def ret_alloc(self):
        A = self.A
        A.push()
        self.cs = A.alloc(2 * S * 4, F32).rearrange("p (a t) -> p a t", t=S)
        self.cs_b = Buf("cs")
        self.QrT = A.alloc(2 * S * 2).rearrange("p (a t) -> p a t", t=S)
        self.KrT = A.alloc(2 * S * 2).rearrange("p (a t) -> p a t", t=S)
        self.QrT_b, self.KrT_b = Buf("QrT"), Buf("KrT")
        self.Krtm = A.alloc(16 * 256 * 2).rearrange("p (c n) -> p c n", n=256)
        self.Krtm_b = Buf("Krtm")
        self.Vtm = A.alloc(16 * 512 * 2).rearrange("p (c n) -> p c n", n=512)
        self.Vtm_b = Buf("Vtm")
        self.YF = A.alloc(16 * 512 * 2).rearrange("p (c n) -> p c n", n=512)
        self.YF_b = Buf("YF")
        self.S32 = A.alloc(1024 * 4, F32)
        self.S32_b = Buf("S32")
        self.Sbf = A.alloc(1024 * 2)
        self.Sbf_b = Buf("Sbf")
        self.tmp_ring = Ring([(A.alloc(2048, F32), Buf(f"tmp{i}")) for i in range(2)])
        self.Qc_ring = Ring([(A.alloc(512).rearrange("p (a t) -> p a t", t=128), Buf(f"Qc{i}")) for i in range(2)])
        self.Kh_ring = Ring([(A.alloc(512), Buf(f"Kh{i}")) for i in range(2)])
        self.AT_ring = Ring([(A.alloc(256), Buf(f"AT{i}")) for i in range(2)])
        self.qs = A.alloc(8 * 128 * 4, F32).rearrange("p (g n) -> p g n", n=128)
        self.Mp = A.alloc(8 * 128 * 4, F32).rearrange("p (g n) -> p g n", n=128)
        self.rc = A.alloc((4 * 128 + 2) * 4, F32)[:, 0:514]
        self.cols = A.alloc(64 * 4, F32)
        self.dec_b = Buf("dec")
        self.rc_b = Buf("rc")
        self.yst_ring = Ring([(A.alloc(1024).rearrange("p (a t) -> p a t", t=128), Buf(f"yst{i}")) for i in range(2)])
        self.ybf_ring = Ring([(A.alloc(1024), Buf(f"ybf{i}")) for i in range(1)])
        self.eg_ring = Ring([(A.alloc(1024), Buf(f"eg{i}")) for i in range(3)])
        A.pop()

    def ret_decay_setup(self, j):
        Sx = self.S
        c = self.cols
        rc = self.rc
        db = self.dec_b
        dl, ee, lg, nlg, gC = c[:, 0:8], c[:, 8:16], c[:, 16:24], c[:, 24:32], c[:, 32:40]
        kd = c[:, 40:48]
        ksc = c[:, 48:56]
        src = self.rdl[j:j + 1, :].partition_broadcast(128)
        Sx.dma("sp", lambda e: e.dma_start(out=dl, in_=src), writes=[db])
        Sx.op("act", lambda e: e.activation(out=ee, in_=dl, func=AF.Exp), reads=[db], writes=[db])
        Sx.op("act", lambda e: e.activation(out=lg, in_=ee, func=AF.Ln, scale=-1.0, bias=1.0), reads=[db], writes=[db])
        Sx.op("act", lambda e: e.activation(out=nlg, in_=lg, func=AF.Copy, scale=-1.0), reads=[db], writes=[db])
        Sx.op("act", lambda e: e.activation(out=gC, in_=lg, func=AF.Exp, scale=128.0), reads=[db], writes=[db])
        for d in range(2):
            for h in range(4):
                i = d * 4 + h
                ramp = rc[:, 256 + d * 128: 256 + (d + 1) * 128]
                colr = rc[:, 512 + d: 513 + d]
                mask = rc[:, d * 128:(d + 1) * 128]
                Sx.op("act", lambda e, i=i, ramp=ramp: e.activation(out=self.qs[:, i, :], in_=ramp, func=AF.Exp,
                                                                    scale=lg[:, i:i + 1]),
                      reads=[db, self.rc_b], writes=[db])
                Sx.op("act", lambda e, i=i, colr=colr: e.activation(out=ksc[:, i:i + 1], in_=colr, func=AF.Exp,
                                                                    scale=nlg[:, i:i + 1]),
                      reads=[db, self.rc_b], writes=[db])
                Sx.op("dve", lambda e, i=i, mask=mask: e.tensor_scalar(out=self.Mp[:, i, :], in0=mask,
                                                                       scalar1=ksc[:, i:i + 1], scalar2=0.0625,
                                                                       op0=ALU.mult, op1=ALU.mult),
                      reads=[db, self.rc_b], writes=[db])
                Sx.op("dve", lambda e, i=i: e.tensor_scalar(out=kd[:, i:i + 1], in0=ksc[:, i:i + 1],
                                                            scalar1=gC[:, i:i + 1], scalar2=0.0625,
                                                            op0=ALU.mult, op1=ALU.mult),
                      reads=[db], writes=[db])

    def ret_run(self, u):
        Sx = self.S
        layer, s, first = u["layer"], u["s"], u["first"]
        j = layer // 2
        if getattr(self, "phase", None) != "ret":
            self.phase_switch("ret")
        gset = 2 * ((layer * 3 + 1) % 2)
        if s == 0:
            self.load_gain(gset, layer, 2)
            self.load_gain(gset + 1, layer, 3)
            self.ret_decay_setup(j)
        for t in range(16):
            self.prenorm_tile(first, s, t, gset)
        hT, ps = self.hT, self.ps
        cs, QrT, KrT, Krtm, Vtm, YF = self.cs, self.QrT, self.KrT, self.Krtm, self.Vtm, self.YF
        S32, Sbf = self.S32, self.Sbf
        S32_3 = S32.rearrange("p (a n) -> p a n", n=512)
        Sbf_3 = Sbf.rearrange("p (a n) -> p a n", n=512)
        c_ = self.cols
        gC, kd = c_[:, 32:40], c_[:, 40:48]
        ident = self.ident
        db = self.dec_b
        nq = 0
        for h in range(4):
            slot_qk, slot_qk_b = self.w_get()
            wqk = slot_qk.rearrange("p (k n) -> p k n", n=512)
            for which, dstT, dst_b in ((0, QrT, self.QrT_b), (1, KrT, self.KrT_b)):
                for tb in range(4):
                    cols = slice(tb * 512, (tb + 1) * 512)

                    def mmq(pe, wqk=wqk, off=which * 256, cols=cols):
                        for dc in range(2):
                            for k in range(8):
                                ins = pe.matmul(ps[:, dc, :], lhsT=wqk[:, k, off + dc * 128: off + (dc + 1) * 128],
                                                rhs=hT[:, k, cols], start=(k == 0), stop=(k == 7))
                        return ins
                    Sx.op("pe", mmq, reads=[slot_qk_b] + self.hT_b[tb * 4:tb * 4 + 4], writes=[self.bank[0], self.bank[1]])
                    ta, ta_b = self.tmp_ring.next()
                    tb_, tb_b = self.tmp_ring.next()
                    t1p, t2p = ps[:, 0, :], ps[:, 1, :]
                    cosb, sinb = cs[:, 0, cols], cs[:, 1, cols]
                    rd = [self.bank[0], self.bank[1], self.cs_b]
                    Sx.op("dve", lambda e, ta=ta, t1p=t1p, cosb=cosb: e.tensor_tensor(out=ta, in0=t1p, in1=cosb, op=ALU.mult),
                          reads=rd, writes=[ta_b])
                    Sx.op("dve", lambda e, tb_=tb_, t2p=t2p, sinb=sinb: e.tensor_tensor(out=tb_, in0=t2p, in1=sinb, op=ALU.mult),
                          reads=rd, writes=[tb_b])
                    Sx.op("dve", lambda e, ta=ta, tb_=tb_, dstT=dstT, cols=cols: e.tensor_tensor(
                        out=dstT[:, 0, cols], in0=ta, in1=tb_, op=ALU.subtract), reads=[ta_b, tb_b], writes=[dst_b])
                    Sx.op("dve", lambda e, ta=ta, t1p=t1p, sinb=sinb: e.tensor_tensor(out=ta, in0=t1p, in1=sinb, op=ALU.mult),
                          reads=rd, writes=[ta_b])
                    Sx.op("dve", lambda e, tb_=tb_, t2p=t2p, cosb=cosb: e.tensor_tensor(out=tb_, in0=t2p, in1=cosb, op=ALU.mult),
                          reads=rd, writes=[tb_b])
                    Sx.op("dve", lambda e, ta=ta, tb_=tb_, dstT=dstT, cols=cols: e.tensor_tensor(
                        out=dstT[:, 1, cols], in0=ta, in1=tb_, op=ALU.add), reads=[ta_b, tb_b], writes=[dst_b])
            pK = ps[:, 7, 0:512].bitcast(BF16).rearrange("p (c n) -> p c n", n=256)
            for c4 in range(4):
                def trk(pe, c4=c4):
                    for cl in range(4):
                        c = c4 * 4 + cl
                        for dc in range(2):
                            ins = pe.transpose(pK[:, cl, dc * 128:(dc + 1) * 128], KrT[:, dc, c * 128:(c + 1) * 128], ident)
                    return ins
                Sx.op("pe", trk, reads=[self.KrT_b, self.ident_b], writes=[self.bank[7]])
                Sx.op("act", lambda e, c4=c4: e.activation(out=Krtm[:, c4 * 4:c4 * 4 + 4, :], in_=pK, func=AF.Copy),
                      reads=[self.bank[7]], writes=[self.Krtm_b])
            slot_v, slot_v_b = self.w_get()
            wv = slot_v.rearrange("p (k n) -> p k n", n=512)
            for c in range(16):
                bank = 2 + c % 2

                def mmv(pe, wv=wv, c=c, bank=bank):
                    for k in range(8):
                        ins = pe.matmul(ps[:, bank, :], lhsT=hT[:, k, c * 128:(c + 1) * 128], rhs=wv[:, k, :],
                                        start=(k == 0), stop=(k == 7))
                    return ins
                Sx.op("pe", mmv, reads=[slot_v_b, self.hT_b[c]], writes=[self.bank[bank]])
                Sx.op("act", lambda e, c=c, bank=bank: e.activation(out=Vtm[:, c, :], in_=ps[:, bank, :], func=AF.Copy),
                      reads=[self.bank[bank]], writes=[self.Vtm_b])
            for d in range(2):
                i8 = d * 4 + h
                slot_g, slot_g_b = self.w_get()
                wgt = slot_g.rearrange("p (k n) -> p k n", n=512)
                Sx.op("dve", lambda e: e.memset(S32, 0.0), writes=[self.S32_b])
                Sx.op("dve", lambda e: e.memset(Sbf, 0.0), writes=[self.Sbf_b])
                order = list(range(16)) if d == 0 else list(range(15, -1, -1))
                stt = {}

                def P0(k, d=d, i8=i8, wgt=wgt, slot_g_b=slot_g_b, order=order, stt=stt):
                    c = order[k]
                    tok = slice(c * 128, (c + 1) * 128)
                    Qc, Qc_b = self.Qc_ring.next()
                    for dc in range(2):
                        Sx.op("dve", lambda e, dc=dc: e.tensor_tensor(out=Qc[:, dc, :], in0=QrT[:, dc, tok], in1=self.qs[:, i8, :],
                                                                      op=ALU.mult),
                              reads=[self.QrT_b, db], writes=[Qc_b])
                    Kh, Kh_b = self.Kh_ring.next()
                    Sx.op("dve", lambda e: e.tensor_scalar(out=Kh, in0=Krtm[:, c, :], scalar1=kd[:, i8:i8 + 1], scalar2=None,
                                                           op0=ALU.mult),
                          reads=[self.Krtm_b, db], writes=[Kh_b])

                    def mma(pe):
                        for dc in range(2):
                            ins = pe.matmul(ps[:, 0, 0:128], lhsT=KrT[:, dc, tok], rhs=Qc[:, dc, :], start=(dc == 0), stop=(dc == 1))
                        return ins
                    Sx.op("pe", mma, reads=[self.KrT_b, Qc_b], writes=[self.bank[0]])
                    gb_ = 2 + k % 2

                    def mmg_(pe):
                        for kk in range(8):
                            ins = pe.matmul(ps[:, gb_, :], lhsT=hT[:, kk, tok], rhs=wgt[:, kk, :], start=(kk == 0), stop=(kk == 7))
                        return ins
                    Sx.op("pe", mmg_, reads=[slot_g_b, self.hT_b[c]], writes=[self.bank[gb_]])

                    def mms(pe):
                        for dc in range(2):
                            ins = pe.matmul(ps[:, 4 + dc, :], lhsT=Kh[:, dc * 128:(dc + 1) * 128], rhs=Vtm[:, c, :], start=True, stop=True)
                        return ins
                    Sx.op("pe", mms, reads=[Kh_b, self.Vtm_b], writes=[self.bank[4], self.bank[5]])
                    stt[k] = dict(c=c, Qc=Qc, Qc_b=Qc_b, gb_=gb_)

                def P1(k, d=d, i8=i8, stt=stt):
                    z = stt[k]
                    c, Qc, Qc_b, gb_ = z["c"], z["Qc"], z["Qc_b"], z["gb_"]
                    AT, AT_b = self.AT_ring.next()
                    Sx.op("dve", lambda e: e.tensor_tensor(out=AT, in0=ps[:, 0, 0:128], in1=self.Mp[:, i8, :], op=ALU.mult),
                          reads=[self.bank[0], db], writes=[AT_b])
                    yb_ = 1 if k % 2 == 0 else 6

                    def mmy(pe):
                        pe.matmul(ps[:, yb_, :], lhsT=AT, rhs=Vtm[:, c, :], start=True, stop=False)
                        for dc in range(2):
                            ins = pe.matmul(ps[:, yb_, :], lhsT=Qc[:, dc, :], rhs=Sbf_3[:, dc, :], start=False, stop=(dc == 1))
                        return ins
                    Sx.op("pe", mmy, reads=[AT_b, Qc_b, self.Vtm_b, self.Sbf_b], writes=[self.bank[yb_]])
                    Sx.op("dve", lambda e: e.scalar_tensor_tensor(out=S32_3, in0=S32_3, scalar=gC[:, i8:i8 + 1],
                                                                  in1=ps[:, 4:6, :], op0=ALU.mult, op1=ALU.add),
                          reads=[self.bank[4], self.bank[5], self.S32_b, db], writes=[self.S32_b])
                    Sx.op("act", lambda e: e.activation(out=Sbf, in_=S32, func=AF.Copy), reads=[self.S32_b], writes=[self.Sbf_b])
                    eg, eg_b = self.eg_ring.next()
                    Sx.op("act", lambda e: e.activation(out=eg, in_=ps[:, gb_, :], func=AF.Silu),
                          reads=[self.bank[gb_]], writes=[eg_b])
                    z.update(yb_=yb_, eg=eg, eg_b=eg_b)

                def P2(k, d=d, h=h, stt=stt):
                    z = stt.pop(k)
                    c, yb_, eg, eg_b = z["c"], z["yb_"], z["eg"], z["eg_b"]
                    st, st_b = self.stat_ring.next()
                    st2, st2_b = self.stat_ring.next()
                    yn, yn_b = self.tmp_ring.next()
                    ypsum = ps[:, yb_, :]
                    Sx.op("act", lambda e: e.activation(out=yn, in_=ypsum, func=AF.Copy, scale=1.0 / 512, accum_out=st2[:, 0:1]),
                          reads=[self.bank[yb_]], writes=[yn_b, st2_b])
                    Sx.op("act", lambda e: e.activation(out=yn, in_=ypsum, func=AF.Square, scale=512.0 ** -0.5, accum_out=st[:, 1:2]),
                          reads=[self.bank[yb_]], writes=[yn_b, st_b])
                    Sx.op("dve", lambda e: e.tensor_tensor(out=st2[:, 1:2], in0=st2[:, 0:1], in1=st2[:, 0:1], op=ALU.mult),
                          reads=[st2_b], writes=[st2_b])
                    Sx.op("dve", lambda e: e.scalar_tensor_tensor(out=st[:, 2:3], in0=st[:, 1:2], scalar=EPS, in1=st2[:, 1:2],
                                                                  op0=ALU.add, op1=ALU.subtract),
                          reads=[st_b, st2_b], writes=[st_b])
                    Sx.op("pool", lambda e: e.tensor_tensor(out=st[:, 3:4], in0=st[:, 2:3], in1=self.mhalf[:, 0:1], op=ALU.pow),
                          reads=[st_b, self.mhalf_b], writes=[st_b])
                    Sx.op("dve", lambda e: e.tensor_scalar(out=yn, in0=ypsum, scalar1=st2[:, 0:1], scalar2=st[:, 3:4],
                                                           op0=ALU.subtract, op1=ALU.mult),
                          reads=[self.bank[yb_], st_b, st2_b], writes=[yn_b])
                    if d == 0:
                        Sx.op("dve", lambda e: e.tensor_tensor(out=YF[:, c, :], in0=yn, in1=eg, op=ALU.mult),
                              reads=[yn_b, eg_b], writes=[self.YF_b])
                    else:
                        Sx.op("dve", lambda e: e.tensor_tensor(out=yn, in0=yn, in1=eg, op=ALU.mult),
                              reads=[yn_b, eg_b], writes=[yn_b])
                        ybf, ybf_b = self.ybf_ring.next()
                        Sx.op("dve", lambda e: e.tensor_tensor(out=ybf, in0=yn, in1=YF[:, c, :], op=ALU.add),
                              reads=[yn_b, self.YF_b], writes=[ybf_b])
                        pY = ps[:, 7, 0:256].bitcast(BF16).rearrange("p (a t) -> p a t", t=128)

                        def try_(pe):
                            for ec in range(4):
                                ins = pe.transpose(pY[:, ec, :], ybf[:, ec * 128:(ec + 1) * 128], ident)
                            return ins
                        Sx.op("pe", try_, reads=[ybf_b, self.ident_b], writes=[self.bank[7]])
                        yst, yst_b = self.yst_ring.next()
                        Sx.op("act", lambda e: e.activation(out=yst, in_=pY, func=AF.Copy), reads=[self.bank[7]], writes=[yst_b])
                        dstd = self.yT_d[s, :, 4 * h:4 * h + 4, c * 128:(c + 1) * 128]
                        Sx.dma("sp", lambda e: e.dma_start(out=dstd, in_=yst), reads=[yst_b], writes=[self.yTd_b[s][c // 4]])

                for it in range(18):
                    if 0 <= it - 1 < 16:
                        P1(it - 1)
                    if it < 16:
                        P0(it)
                    if 0 <= it - 2 < 16:
                        P2(it - 2)
        wsl = [self.w_get(hold=i) for i in range(4)]
        yTl = Vtm
        for tb in range(4):
            srcd = self.yT_d[s, :, :, tb * 512:(tb + 1) * 512]
            Sx.dma("sp", lambda e, srcd=srcd: e.dma_start(out=yTl, in_=srcd), reads=[self.yTd_b[s][tb]], writes=[self.Vtm_b])
            for tt in range(4):
                t = tb * 4 + tt
                b0 = 2 * (t % 2)

                def mmo(pe, tt=tt, b0=b0):
                    for dh in range(2):
                        for kc in range(16):
                            w3 = wsl[dh * 2 + kc // 8][0].rearrange("p (k n) -> p k n", n=512)
                            ins = pe.matmul(ps[:, b0 + dh, :], lhsT=yTl[:, kc, tt * 128:(tt + 1) * 128], rhs=w3[:, kc % 8, :],
                                            start=(kc == 0), stop=(kc == 15))
                    return ins
                Sx.op("pe", mmo, reads=[w_[1] for w_ in wsl] + [self.Vtm_b], writes=[self.bank[b0], self.bank[b0 + 1]])
                self.postnorm_tile(first, s, t, b0, gset + 1, 1.0)

    def phase_switch(self, name):
        fence = {}
        for e in ("pe", "act", "dve", "pool"):
            if self.S.tick[e] > 0:
                fence[e] = self.S.tick[e]
        for q in ("sp", "pool", "act"):
            for i in range(DMA_K):
                if self.S.pool_target[q][i] > 0:
                    fence[("dma", q, i)] = self.S.pool_target[q][i]
        self.phase = name
        if name == "ffn":
            self.ffn_alloc()
            locs = self.aT_b + [self.Wd_b] + [b for _, b in self.sg_ring.items]
        elif name == "attn":
            self.attn_alloc()
            locs = ([self.oT_b, self.qT_b, self.kT_b, self.E_b] + self.acc_b + self.den_b + self.Vaug_b
                    + [b for _, b in self.pe32_ring.items] + [b for _, b in self.PT_ring.items])
        elif name == "ret":
            self.ret_alloc()
            locs = ([self.cs_b, self.QrT_b, self.KrT_b, self.Krtm_b, self.Vtm_b, self.YF_b, self.S32_b, self.Sbf_b,
                     self.dec_b, self.rc_b]
                    + [b for rg in (self.tmp_ring, self.Qc_ring, self.Kh_ring, self.AT_ring, self.yst_ring, self.ybf_ring,
                                    self.eg_ring)
                       for _, b in rg.items])
        for bf in locs:
            bf.r = dict(fence)
        if name == "attn":
            self.attn_setup()
        if name == "ret":
            self.S.dma("sp", lambda e: e.dma_start(out=self.cs, in_=self.cs_d), writes=[self.cs_b])
            self.S.dma("sp", lambda e: e.dma_start(out=self.rc, in_=self.rconst), writes=[self.rc_b])


FULL_PLAN = [(l, sub) for l in range(NL) for sub in range(3)]


def t5_buckets(rel):
    half = 16
    max_exact = 8
    n = np.abs(rel)
    large = max_exact + (np.log(np.maximum(n, 1) / max_exact) / np.log(1024 / max_exact) * (half - max_exact)).astype(np.int64)
    large = np.minimum(large, half - 1)
    return ((rel > 0) * half + np.where(n < max_exact, n, large)).astype(np.int32)


def expand_bias(rel_bias):
    kp = np.arange(128)[:, None]
    qi = np.arange(256)[None, :]
    off = kp - qi + 64
    valid = np.abs(off) <= 64
    out = np.full((128, 48, 256), -30000.0, dtype=np.float32)
    for g, dil in enumerate((1, 4, 16)):
        bk = t5_buckets(np.clip(off, -64, 64) * dil)
        for h in range(16):
            tile = rel_bias[g * 16 + h][bk]
            out[:, g * 16 + h, :] = np.where(valid, tile, np.float32(-30000.0))
    return out


def ret_consts():
    jj = np.arange(128)[:, None]
    ii = np.arange(128)[None, :]
    maskf = (ii >= jj).astype(np.float32)
    maskb = (jj >= ii).astype(np.float32)
    rampf = np.broadcast_to((ii + 1).astype(np.float32), (128, 128))
    rampb = np.broadcast_to((128 - ii).astype(np.float32), (128, 128))
    colr = np.concatenate([(jj + 1), (128 - jj)], axis=1).astype(np.float32)
    rconst = np.ascontiguousarray(np.concatenate([maskf, maskb, rampf, rampb, colr], axis=1), dtype=np.float32)
    inv_freq = (1.0 / (np.float32(10000.0) ** np.linspace(0.0, 1.0, 128, dtype=np.float32))).astype(np.float32)
    ang = (np.arange(S, dtype=np.float32)[None, :] * inv_freq[:, None]).astype(np.float32)
    cossin = np.stack([np.cos(ang), np.sin(ang)], axis=1).astype(np.float32)
    return rconst, np.ascontiguousarray(cossin)


def run_plan(inputs, plan, nseq, ncores, trace=False, debug_out=None):
    prog = Prog(nseq, plan, debug_out)
    nc = prog.build()
    x = np.ascontiguousarray(inputs["x"], dtype=np.float32)
    ident = np.eye(128, dtype=np.float32)
    biasexp = expand_bias(np.asarray(inputs["rel_bias"], dtype=np.float32))
    rconst, cossin = ret_consts()
    in_maps = []
    for c in range(ncores):
        m = {"x": x[c * nseq:(c + 1) * nseq].reshape(nseq * S, D),
             "norm_gains": np.ascontiguousarray(inputs["norm_gains"], dtype=np.float32),
             "ffn_w_gate": np.ascontiguousarray(inputs["ffn_w_gate"], dtype=np.float32),
             "ffn_w_up": np.ascontiguousarray(inputs["ffn_w_up"], dtype=np.float32),
             "ffn_w_down": np.ascontiguousarray(inputs["ffn_w_down"], dtype=np.float32),
             "ident": ident,
             "attn_w_in": np.ascontiguousarray(inputs["attn_w_in"], dtype=np.float32),
             "attn_w_out": np.ascontiguousarray(inputs["attn_w_out"], dtype=np.float32),
             "biasexp": biasexp,
             "ret_w_in": np.ascontiguousarray(inputs["ret_w_in"], dtype=np.float32),
             "ret_w_out": np.ascontiguousarray(inputs["ret_w_out"], dtype=np.float32),
             "ret_decay_logit": np.ascontiguousarray(inputs["ret_decay_logit"], dtype=np.float32).reshape(2, 8),
             "rconst": rconst, "cossin": cossin}
        in_maps.append(m)
    res = run_bass_kernel_spmd(nc, in_maps, core_ids=list(range(ncores)), trace=trace)
    out = np.stack([r["y"].reshape(nseq, S, D) for r in res.results], axis=0).reshape(ncores * nseq, S, D)
    if debug_out is not None:
        return out, res, {k: res.results[0][k] for k in prog.dbg_names}
    return out, res


def kernel(**inputs):
    out, _ = run_plan(inputs, FULL_PLAN, 2, NCORES)
    return out.astype(np.float32)
```

```python
import contextlib
import numpy as np
import concourse.bass as bass
import concourse.mybir as mybir
from concourse.bass_utils import run_bass_kernel_spmd

F32 = mybir.dt.float32
BF16 = mybir.dt.bfloat16
AF = mybir.ActivationFunctionType
ALU = mybir.AluOpType

D = 1024
S = 2048
DFF = 2816
NCH = DFF // 128
NL = 4
EPS = 1e-6
NCORES = 8
NSLOT = 4
DMA_K = 6


class Buf:
    __slots__ = ("name", "w", "r")

    def __init__(self, name):
        self.name = name
        self.w = None
        self.r = {}


class Sched:
    ENG = ("pe", "act", "dve", "pool", "sp")

    def __init__(self):
        self.streams = {e: [] for e in self.ENG}
        self.tick = {e: 0 for e in self.ENG}
        self.waited = {e: {} for e in self.ENG}
        self.pool_next = {q: 0 for q in ("sp", "pool", "act")}
        self.pool_target = {q: [0] * DMA_K for q in ("sp", "pool", "act")}

    def _waits(self, eng, reads, writes):
        need = {}

        def add(sem, val):
            if need.get(sem, 0) < val:
                need[sem] = val

        for b in reads:
            if b.w is not None:
                add(*b.w)
        for b in writes:
            if b.w is not None:
                add(*b.w)
            for sem, val in b.r.items():
                add(sem, val)
        out = []
        wd = self.waited[eng]
        for sem, val in need.items():
            if sem == "pe" and eng == "pe":
                continue
            if wd.get(sem, 0) >= val:
                continue
            wd[sem] = val
            out.append((sem, val))
        return out

    @staticmethod
    def _update(ev, reads, writes):
        sem, val = ev
        for b in reads:
            if b.r.get(sem, 0) < val:
                b.r[sem] = val
        for b in writes:
            b.w = ev
            b.r = {}

    def op(self, eng, fn, reads=(), writes=()):
        waits = self._waits(eng, reads, writes)
        self.tick[eng] += 1
        ev = (eng, self.tick[eng])
        self.streams[eng].append((waits, fn, ev, 1))
        self._update(ev, reads, writes)
        return ev

    def dma(self, q, fn, reads=(), writes=()):
        i = self.pool_next[q]
        self.pool_next[q] = (i + 1) % DMA_K
        semkey = ("dma", q, i)
        waits = self._waits(q, reads, writes)
        prev = self.pool_target[q][i]
        if prev > 0 and self.waited[q].get(semkey, 0) < prev:
            self.waited[q][semkey] = prev
            waits.append((semkey, prev))
        self.pool_target[q][i] = prev + 16
        ev = (semkey, prev + 16)
        self.streams[q].append((waits, fn, ev, 16))
        self._update(ev, reads, writes)
        return ev

    def sem_keys(self):
        keys = [e for e in self.ENG if e != "sp"]
        for q in ("sp", "pool", "act"):
            for i in range(DMA_K):
                keys.append(("dma", q, i))
        return keys

    def emit(self, nc, block, semh):
        final = []
        for q in ("sp", "pool", "act"):
            for i in range(DMA_K):
                if self.pool_target[q][i] > 0:
                    final.append((("dma", q, i), self.pool_target[q][i]))
        for e in ("pe", "act", "dve", "pool"):
            if self.tick[e] > 0:
                final.append((e, self.tick[e]))

        def run(eng, stream, tail=False):
            for waits, fn, ev, inc in stream:
                for sem, val in waits:
                    eng.wait_ge(semh[sem], val)
                ins = fn(eng)
                ins.then_inc(semh[ev[0]], inc)
            if tail:
                for sem, val in final:
                    eng.wait_ge(semh[sem], val)

        @block.tensor
        def _(pe):
            run(pe, self.streams["pe"])

        @block.scalar
        def _(act):
            run(act, self.streams["act"])

        @block.vector
        def _(dve):
            run(dve, self.streams["dve"])

        @block.gpsimd
        def _(pool):
            run(pool, self.streams["pool"])

        @block.sync
        def _(sp):
            run(sp, self.streams["sp"], tail=True)


class Ring:
    def __init__(self, items):
        self.items = items
        self.i = 0

    def next(self):
        it = self.items[self.i]
        self.i = (self.i + 1) % len(self.items)
        return it


class Arena:
    def __init__(self, ap, nbytes):
        self.ap = ap
        self.nbytes = nbytes
        self.off = 0
        self.marks = []

    def alloc(self, nbytes, dtype=BF16):
        nbytes = (nbytes + 63) // 64 * 64
        assert self.off + nbytes <= self.nbytes, ("SBUF arena overflow", self.off, nbytes, self.nbytes)
        a = self.ap[:, self.off // 2:(self.off + nbytes) // 2]
        self.off += nbytes
        if dtype == F32:
            a = a.bitcast(F32)
        return a

    def push(self):
        self.marks.append(self.off)

    def pop(self):
        self.off = self.marks.pop()


class Prog:
    def __init__(self, nseq, plan, debug_out=None):
        self.nseq = nseq
        self.plan = plan
        self.debug_out = debug_out
        self.dbg_names = []
        self.S = Sched()
        self.nc = bass.Bass("TRN2", target_bir_lowering=False)
        nc = self.nc
        T = nseq * S
        self.x_in = nc.dram_tensor("x", [T, D], F32, kind="ExternalInput").ap()
        self.y = nc.dram_tensor("y", [T, D], F32, kind="ExternalOutput").ap()
        self.gains = nc.dram_tensor("norm_gains", [NL, 6, D], F32, kind="ExternalInput").ap()
        self.wg = nc.dram_tensor("ffn_w_gate", [NL, 2, D, DFF], F32, kind="ExternalInput").ap()
        self.wu = nc.dram_tensor("ffn_w_up", [NL, 2, D, DFF], F32, kind="ExternalInput").ap()
        self.wd = nc.dram_tensor("ffn_w_down", [NL, 2, DFF, D], F32, kind="ExternalInput").ap()
        self.ident_d = nc.dram_tensor("ident", [128, 128], F32, kind="ExternalInput").ap()
        self.awin = nc.dram_tensor("attn_w_in", [2, D, 9216], F32, kind="ExternalInput").ap()
        self.awout = nc.dram_tensor("attn_w_out", [2, D, D], F32, kind="ExternalInput").ap()
        self.biasexp = nc.dram_tensor("biasexp", [128, 48, 256], F32, kind="ExternalInput").ap()
        self.rwin = nc.dram_tensor("ret_w_in", [2, D, 8192], F32, kind="ExternalInput").ap()
        self.rwout = nc.dram_tensor("ret_w_out", [2, 2048, D], F32, kind="ExternalInput").ap()
        self.rdl = nc.dram_tensor("ret_decay_logit", [2, 8], F32, kind="ExternalInput").ap()
        self.rconst = nc.dram_tensor("rconst", [128, 4 * 128 + 2], F32, kind="ExternalInput").ap()
        self.cs_d = nc.dram_tensor("cossin", [128, 2, S], F32, kind="ExternalInput").ap()
        self.yT_d = nc.dram_tensor("yT_scratch", [nseq, 128, 16, S], BF16, kind="Internal").ap()

    def debug(self, name, ap, reads):
        if self.debug_out is None or name not in self.debug_out:
            return
        shape = list(ap.shape)
        d = self.nc.dram_tensor("dbg_" + name, shape, F32, kind="ExternalOutput").ap()
        self.S.dma("pool", lambda e: e.dma_start(out=d, in_=ap), reads=reads)
        self.dbg_names.append("dbg_" + name)

    def bufs(self, name, n):
        return [Buf(f"{name}{i}") for i in range(n)]

    def build(self):
        nc = self.nc
        Sx = self.S
        with contextlib.ExitStack() as es:
            ARENA_BYTES = 207 * 1024
            arena_t = es.enter_context(nc.sbuf_tensor("arena", [128, ARENA_BYTES // 2], BF16))
            ps_t = es.enter_context(nc.psum_tensor("ps", [128, 8, 512], F32))
            self.ps = ps_t
            self.bank = self.bufs("bank", 8)
            A = Arena(arena_t, ARENA_BYTES)
            self.A = A
            self.ident = A.alloc(256)
            self.ident_b = Buf("ident")
            self.mhalf = A.alloc(64, F32)
            self.mhalf_b = Buf("mhalf")
            Sx.op("pool", lambda e: e.memset(self.mhalf, -0.5), writes=[self.mhalf_b])
            self.stat = A.alloc(16 * 16, F32)
            self.stat_ring = Ring([(self.stat[:, 4 * i:4 * i + 4], Buf(f"stat{i}")) for i in range(16)])
            self.gain = [(A.alloc(4096, F32), Buf(f"gain{i}")) for i in range(4)]
            self.xt_ring = Ring([(A.alloc(4096, F32), Buf(f"xt{i}")) for i in range(3)])
            self.hb_ring = Ring([(A.alloc(2048), Buf(f"hb{i}")) for i in range(2)])
            self.t1_ring = Ring([(A.alloc(4096, F32), Buf(f"t1_{i}")) for i in range(2)])
            self.hT = A.alloc(8 * S * 2).rearrange("p (k t) -> p k t", t=S)
            self.hT_b = self.bufs("hT", 16)
            self.wring = [(A.alloc(8192), Buf(f"wslot{i}")) for i in range(NSLOT)]
            self.x_b = [self.bufs(f"x{s}_", 16) for s in range(self.nseq)]
            self.yTd_b = [self.bufs(f"yTd{s}_", 4) for s in range(self.nseq)]
            self.x_first = True

            Sx.dma("pool", lambda e: e.dma_start(out=self.ident, in_=self.ident_d), writes=[self.ident_b])

            units = self.make_units()
            self.slabs = []
            for u in units:
                u["slab0"] = len(self.slabs)
                self.slabs.extend(u["slabs"])
            self.slab_issued = 0
            self.slab_next = 0
            for u in units:
                u["run"](u)

            sem_keys = Sx.sem_keys()
            semh = {}
            for i, k in enumerate(sem_keys):
                semh[k] = es.enter_context(nc.semaphore(f"s{i}"))
            block = es.enter_context(nc.Block())
            Sx.emit(nc, block, semh)
        return nc

    def w_issue_upto(self, idx):
        while self.slab_issued <= min(idx, len(self.slabs) - 1):
            i = self.slab_issued
            slot_ap, slot_b = self.wring[i % NSLOT]
            for (dst_fn, src) in self.slabs[i]:
                dst = dst_fn(slot_ap)
                self.S.dma("pool", (lambda d, s_: (lambda e: e.dma_start(out=d, in_=s_)))(dst, src), writes=[slot_b])
            self.slab_issued += 1

    def w_get(self, hold=0):
        i = self.slab_next
        self.slab_next += 1
        self.w_issue_upto(i + NSLOT - 1 - hold)
        return self.wring[i % NSLOT]

    def x_rows(self, first, s, t):
        src = self.x_in if first else self.y
        r0 = s * S + t * 128
        return src[r0:r0 + 128, :]

    def load_gain(self, slot, layer, idx):
        g_ap, g_b = self.gain[slot]
        src = self.gains[layer, idx:idx + 1, :].partition_broadcast(128)
        self.S.dma("sp", lambda e: e.dma_start(out=g_ap, in_=src), writes=[g_b])

    def rstd_ops(self, st, st_b, n):
        Sx = self.S
        Sx.op("dve", lambda e: e.tensor_scalar(out=st[:, 1:2], in0=st[:, 0:1], scalar1=1.0 / n, scalar2=EPS,
                                               op0=ALU.mult, op1=ALU.add),
              reads=[st_b], writes=[st_b])
        Sx.op("pool", lambda e: e.tensor_tensor(out=st[:, 2:3], in0=st[:, 1:2], in1=self.mhalf[:, 0:1], op=ALU.pow),
              reads=[st_b, self.mhalf_b], writes=[st_b])

    def prenorm_front(self, first, s, t, gslot):
        Sx = self.S
        xs, xs_b = self.xt_ring.next()
        hb, hb_b = self.hb_ring.next()
        st, st_b = self.stat_ring.next()
        g_ap, g_b = self.gain[gslot]
        src = self.x_rows(first, s, t)
        Sx.dma("sp", lambda e: e.dma_start(out=xs, in_=src), reads=[self.x_b[s][t]], writes=[xs_b])
        Sx.op("act", lambda e: e.activation(out=hb, in_=xs, func=AF.Square, accum_out=st[:, 0:1]),
              reads=[xs_b], writes=[hb_b, st_b])
        self.rstd_ops(st, st_b, D)
        Sx.op("dve", lambda e: e.scalar_tensor_tensor(out=hb, in0=xs, scalar=st[:, 2:3], in1=g_ap,
                                                      op0=ALU.mult, op1=ALU.mult),
              reads=[xs_b, st_b, g_b], writes=[hb_b])
        return hb, hb_b

    def prenorm_back(self, ctx, t):
        Sx = self.S
        hb, hb_b = ctx
        pT = self.ps[:, 7, 0:512].bitcast(BF16).rearrange("p (k t) -> p k t", t=128)
        ident = self.ident

        def tr(pe):
            for k in range(8):
                ins = pe.transpose(pT[:, k, :], hb[:, k * 128:(k + 1) * 128], ident)
            return ins
        Sx.op("pe", tr, reads=[hb_b, self.ident_b], writes=[self.bank[7]])
        hT = self.hT
        Sx.op("act", lambda e: e.activation(out=hT[:, :, t * 128:(t + 1) * 128], in_=pT, func=AF.Copy),
              reads=[self.bank[7]], writes=[self.hT_b[t]])

    def prenorm_tiles(self, first, s, tiles, gslot):
        prev = None
        for t in tiles:
            ctx = self.prenorm_front(first, s, t, gslot)
            if prev is not None:
                self.prenorm_back(*prev)
            prev = (ctx, t)
        self.prenorm_back(*prev)

    def postnorm_tile(self, first, s, t, b0, gslot, coef):
        Sx = self.S
        m = self.ps[:, b0:b0 + 2, :].rearrange("p a b -> p (a b)")
        mb = [self.bank[b0], self.bank[b0 + 1]]
        t1, t1_b = self.t1_ring.next()
        st, st_b = self.stat_ring.next()
        xs, xs_b = self.xt_ring.next()
        g_ap, g_b = self.gain[gslot]
        src = self.x_rows(first, s, t)
        dst = self.y[s * S + t * 128: s * S + t * 128 + 128, :]
        Sx.dma("sp", lambda e: e.dma_start(out=xs, in_=src), reads=[self.x_b[s][t]], writes=[xs_b])
        Sx.op("act", lambda e: e.activation(out=t1, in_=m, func=AF.Square, accum_out=st[:, 0:1]),
              reads=mb, writes=[t1_b, st_b])
        self.rstd_ops(st, st_b, D)
        Sx.op("dve", lambda e: e.scalar_tensor_tensor(out=t1, in0=m, scalar=st[:, 2:3], in1=g_ap,
                                                      op0=ALU.mult, op1=ALU.mult),
              reads=mb + [st_b, g_b], writes=[t1_b])
        Sx.op("dve", lambda e: e.scalar_tensor_tensor(out=xs, in0=t1, scalar=float(coef), in1=xs,
                                                      op0=ALU.mult, op1=ALU.add),
              reads=[t1_b, xs_b], writes=[xs_b])
        Sx.dma("sp", lambda e: e.dma_start(out=dst, in_=xs), reads=[xs_b], writes=[self.x_b[s][t]])

    def make_units(self):
        units = []
        for (layer, sub) in self.plan:
            if sub in (0, 2):
                f = 0 if sub == 0 else 1
                for s in range(self.nseq):
                    for b in range(2):
                        units.append(self.ffn_unit(layer, f, s, b))
            elif layer % 2 == 0:
                for s in range(self.nseq):
                    units.append(self.attn_unit(layer, s))
            else:
                for s in range(self.nseq):
                    units.append(self.ret_unit(layer, s))
        return units

    def ffn_unit(self, layer, f, s, b):
        wg, wu = self.wg[layer, f], self.wu[layer, f]
        slabs = []
        for j in range(NCH // 2):
            c0 = j * 256
            gsrc = wg[:, c0:c0 + 256].rearrange("(k p) n -> p k n", p=128)
            usrc = wu[:, c0:c0 + 256].rearrange("(k p) n -> p k n", p=128)
            slabs.append([
                (lambda sl: sl.rearrange("p (k n) -> p k n", n=512)[:, :, 0:256], gsrc),
                (lambda sl: sl.rearrange("p (k n) -> p k n", n=512)[:, :, 256:512], usrc),
            ])
        first = (layer, 0 if f == 0 else 2) == tuple(self.plan[0])
        return dict(kind="ffn", layer=layer, f=f, s=s, b=b, slabs=slabs, run=self.ffn_run, first=first)

    def ffn_alloc(self):
        A = self.A
        A.push()
        self.aT = A.alloc(NCH * 1024 * 2).rearrange("p (c t) -> p c t", t=1024)
        self.aT_b = self.bufs("aT", 2)
        self.Wd = A.alloc(NCH * 1024 * 2).rearrange("p (c n) -> p c n", n=1024)
        self.Wd_b = Buf("Wd")
        self.sg_ring = Ring([(A.alloc(2048, F32), Buf(f"sg{i}")) for i in range(2)])
        self.Wd_loaded = None
        A.pop()

    def ffn_run(self, u):
        Sx = self.S
        layer, f, s, b = u["layer"], u["f"], u["s"], u["b"]
        first = u["first"]
        if getattr(self, "phase", None) != "ffn":
            self.phase_switch("ffn")
        gset = 2 * ((layer * 3 + 2 * f) % 2)
        if s == 0 and b == 0:
            self.load_gain(gset, layer, 0 if f == 0 else 4)
            self.load_gain(gset + 1, layer, 1 if f == 0 else 5)
        self.prenorm_tiles(first, s, range(8 * b, 8 * b + 8), gset)
        hT, aT, Wd, ps = self.hT, self.aT, self.Wd, self.ps
        n = 0
        for j in range(NCH // 2):
            slot, slot_b = self.w_get()
            if j == 0 and self.Wd_loaded != (layer, f):
                self.Wd_loaded = (layer, f)
                wd = self.wd[layer, f]
                for q in range(2):
                    src = wd[q * 1408:(q + 1) * 1408, :].rearrange("(c p) n -> p c n", p=128)
                    dstw = Wd[:, q * 11:(q + 1) * 11, :]
                    Sx.dma("pool", (lambda d_, s_: (lambda e: e.dma_start(out=d_, in_=s_)))(dstw, src),
                           writes=[self.Wd_b])
            w3 = slot.rearrange("p (k n) -> p k n", n=512)
            for cc in range(2):
                c = 2 * j + cc
                for tb in range(2):
                    col0 = b * 1024 + tb * 512
                    G, U = n % 2, 2 + n % 2
                    n += 1
                    hbufs = self.hT_b[col0 // 128: col0 // 128 + 4]

                    def mmg(pe, off=cc * 128, col0=col0, bank=G, w3=w3):
                        for k in range(8):
                            ins = pe.matmul(ps[:, bank, :], lhsT=w3[:, k, off:off + 128], rhs=hT[:, k, col0:col0 + 512],
                                            start=(k == 0), stop=(k == 7))
                        return ins
                    Sx.op("pe", mmg, reads=[slot_b] + hbufs, writes=[self.bank[G]])

                    def mmu(pe, off=256 + cc * 128, col0=col0, bank=U, w3=w3):
                        for k in range(8):
                            ins = pe.matmul(ps[:, bank, :], lhsT=w3[:, k, off:off + 128], rhs=hT[:, k, col0:col0 + 512],
                                            start=(k == 0), stop=(k == 7))
                        return ins
                    Sx.op("pe", mmu, reads=[slot_b] + hbufs, writes=[self.bank[U]])
                    sg, sg_b = self.sg_ring.next()
                    Sx.op("act", lambda e, sg=sg, bank=G: e.activation(out=sg, in_=ps[:, bank, :], func=AF.Silu),
                          reads=[self.bank[G]], writes=[sg_b])
                    Sx.op("dve", lambda e, sg=sg, bank=U, c=c, tb=tb: e.tensor_tensor(
                        out=aT[:, c, tb * 512:(tb + 1) * 512], in0=sg, in1=ps[:, bank, :], op=ALU.mult),
                        reads=[sg_b, self.bank[U]], writes=[self.aT_b[tb]])
        if b == 0 and s == 0:
            self.debug("hT", hT[:, :, 0:1024], self.hT_b[0:8])
            self.debug("aT", aT, self.aT_b)
            self.debug("Wd", Wd, [self.Wd_b])
        for tt in range(8):
            b0 = 4 + 2 * (tt % 2)

            def mmd(pe, tt=tt, b0=b0):
                for h in range(2):
                    for c in range(NCH):
                        ins = pe.matmul(ps[:, b0 + h, :], lhsT=aT[:, c, tt * 128:(tt + 1) * 128],
                                        rhs=Wd[:, c, h * 512:(h + 1) * 512], start=(c == 0), stop=(c == NCH - 1))
                return ins
            Sx.op("pe", mmd, reads=[self.aT_b[tt // 4], self.Wd_b], writes=[self.bank[b0], self.bank[b0 + 1]])
            self.postnorm_tile(first, s, 8 * b + tt, b0, gset + 1, 0.5)

    def attn_unit(self, layer, s):
        j = layer // 2
        win = self.awin[j]
        slabs = []
        for p in range(8):
            for g in range(3):
                parts = []
                for qi in range(3):
                    c0 = g * 3072 + qi * 1024 + p * 128
                    src = win[:, c0:c0 + 128].rearrange("(k p) n -> p k n", p=128)
                    parts.append(((lambda sl, qi=qi: sl.rearrange("p (k n) -> p k n", n=512)[:, :, qi * 128:(qi + 1) * 128]), src))
                slabs.append(parts)
        wout = self.awout[j]
        for h in range(2):
            src = wout[:, h * 512:(h + 1) * 512].rearrange("(k p) n -> p k n", p=128)
            slabs.append([((lambda sl: sl.rearrange("p (k n) -> p k n", n=512)), src)])
        first = (layer, 1) == tuple(self.plan[0])
        return dict(kind="attn", layer=layer, s=s, slabs=slabs, run=self.attn_run, first=first)

    def attn_alloc(self):
        A = self.A
        A.push()
        self.oT = A.alloc(8 * S * 2).rearrange("p (k t) -> p k t", t=S)
        self.oT_b = Buf("oT")
        self.acc = [A.alloc(S * 4, F32), A.alloc(S * 4, F32)]
        self.acc_b = self.bufs("acc", 2)
        self.den = A.alloc(S * 4, F32)
        self.den_b = self.bufs("den", 2)
        self.qT = A.alloc(S * 2)
        self.kT = A.alloc(S * 2)
        self.qT_b, self.kT_b = Buf("qT"), Buf("kT")
        self.Vaug = [A.alloc(16 * 128 * 2).rearrange("p (c n) -> p c n", n=128) for _ in range(2)]
        self.Vaug_b = self.bufs("Vaug", 2)
        self.E = A.alloc(48 * 256 * 2).rearrange("p (g n) -> p g n", n=256)
        self.E_b = Buf("E")
        self.pe32_ring = Ring([(A.alloc(1024, F32), Buf(f"pe32_{i}")) for i in range(2)])
        self.PT_ring = Ring([(A.alloc(512), Buf(f"PT{i}")) for i in range(2)])
        A.pop()

    def attn_setup(self):
        Sx = self.S
        E = self.E
        for q in range(4):
            stage = self.acc[0] if q % 2 == 0 else self.acc[1]
            stage_b = self.acc_b[q % 2]
            st3 = stage.rearrange("p (g n) -> p g n", n=256)[:, 0:8, :]
            src = self.biasexp[:, q * 12:q * 12 + 8, :]
            Sx.dma("sp", lambda e, st3=st3, src=src: e.dma_start(out=st3, in_=src), writes=[stage_b])
            Sx.op("act", lambda e, st3=st3, q=q: e.activation(out=E[:, q * 12:q * 12 + 8, :], in_=st3, func=AF.Exp),
                  reads=[stage_b], writes=[self.E_b])
            st4 = stage.rearrange("p (g n) -> p g n", n=256)[:, 0:4, :]
            src2 = self.biasexp[:, q * 12 + 8:q * 12 + 12, :]
            Sx.dma("sp", lambda e, st4=st4, src2=src2: e.dma_start(out=st4, in_=src2), reads=[], writes=[stage_b])
            Sx.op("act", lambda e, st4=st4, q=q: e.activation(out=E[:, q * 12 + 8:q * 12 + 12, :], in_=st4, func=AF.Exp),
                  reads=[stage_b], writes=[self.E_b])
        for i in range(2):
            Sx.op("dve", lambda e, i=i: e.memset(self.Vaug[i], 1.0), writes=[self.Vaug_b[i]])

    def attn_run(self, u):
        Sx = self.S
        layer, s, first = u["layer"], u["s"], u["first"]
        if getattr(self, "phase", None) != "attn":
            self.phase_switch("attn")
        gset = 2 * ((layer * 3 + 1) % 2)
        if s == 0:
            self.load_gain(gset, layer, 2)
            self.load_gain(gset + 1, layer, 3)
        self.prenorm_tiles(first, s, range(16), gset)
        hT, ps, oT = self.hT, self.ps, self.oT
        qT, kT = self.qT, self.kT
        nproj = 0
        nv = 0
        nst = 0
        for p in range(8):
            for i in range(2):
                Sx.op("dve", lambda e, i=i: e.memset(self.acc[i], 0.0), writes=[self.acc_b[i]])
            for g in range(3):
                r = (1, 4, 16)[g]
                Ls = S // r
                nb = Ls // 128
                slot, slot_b = self.w_get()
                w3 = slot.rearrange("p (k n) -> p k n", n=512)
                for which, dstT, dst_b, scale in ((0, qT, self.qT_b, 0.125), (1, kT, self.kT_b, 1.0)):
                    for tb in range(4):
                        bank = nproj % 2
                        nproj += 1

                        def mmp(pe, w3=w3, off=which * 128, tb=tb, bank=bank):
                            for k in range(8):
                                ins = pe.matmul(ps[:, bank, :], lhsT=w3[:, k, off:off + 128],
                                                rhs=hT[:, k, tb * 512:(tb + 1) * 512], start=(k == 0), stop=(k == 7))
                            return ins
                        Sx.op("pe", mmp, reads=[slot_b] + self.hT_b[tb * 4:tb * 4 + 4], writes=[self.bank[bank]])
                        Sx.op("act", lambda e, dstT=dstT, tb=tb, bank=bank, scale=scale: e.activation(
                            out=dstT[:, tb * 512:(tb + 1) * 512], in_=ps[:, bank, :], func=AF.Copy, scale=scale),
                            reads=[self.bank[bank]], writes=[dst_b])
                hT4 = hT.rearrange("p k (t r) -> p k t r", r=r)
                for c4 in range(4):
                    bank = 2 + nv % 2
                    nv += 1
                    pv3 = ps[:, bank, :].rearrange("p (c n) -> p c n", n=128)

                    def mmv(pe, w3=w3, c4=c4, pv3=pv3, hT4=hT4, nb=nb):
                        for cl in range(4):
                            ci = c4 * 4 + cl
                            res, jb = ci // nb, ci % nb
                            for k in range(8):
                                ins = pe.matmul(pv3[:, cl, :], lhsT=hT4[:, k, jb * 128:(jb + 1) * 128, res],
                                                rhs=w3[:, k, 256:384], start=(k == 0), stop=(k == 7))
                        return ins
                    Sx.op("pe", mmv, reads=[slot_b] + self.hT_b, writes=[self.bank[bank]])
                    Sx.op("act", lambda e, c4=c4, pv3=pv3: e.activation(
                        out=self.Vaug[0][:, c4 * 4:c4 * 4 + 4, 0:64], in_=pv3[:, :, 0:64], func=AF.Copy),
                        reads=[self.bank[bank]], writes=[self.Vaug_b[0]])
                    Sx.op("act", lambda e, c4=c4, pv3=pv3: e.activation(
                        out=self.Vaug[1][:, c4 * 4:c4 * 4 + 4, 64:128], in_=pv3[:, :, 64:128], func=AF.Copy),
                        reads=[self.bank[bank]], writes=[self.Vaug_b[1]])
                q3 = qT.rearrange("p (t r) -> p t r", r=r)
                k3 = kT.rearrange("p (t r) -> p t r", r=r)
                work = [(e_, ci) for e_ in range(2) for ci in range(16)]
                pend = None

                def st_stage(e_, ci):
                    nonlocal nst
                    res, jb = ci // nb, ci % nb
                    qlo, qhi = max(0, 128 * jb - 64), min(Ls, 128 * jb + 192)
                    nq = qhi - qlo
                    qi0 = qlo - (128 * jb - 64)
                    bank = 4 + nst % 2
                    nst += 1
                    hp = slice(64 * e_, 64 * e_ + 64)
                    l_ap = k3[hp, jb * 128:(jb + 1) * 128, res]
                    r_ap = q3[hp, qlo:qhi, res]
                    Sx.op("pe", lambda pe, bank=bank: pe.matmul(
                        ps[:, bank, 0:nq], lhsT=l_ap, rhs=r_ap, start=True, stop=True),
                        reads=[self.qT_b, self.kT_b], writes=[self.bank[bank]])
                    pe32, pe32_b = self.pe32_ring.next()
                    PT, PT_b = self.PT_ring.next()
                    Sx.op("act", lambda e, bank=bank: e.activation(out=pe32[:, 0:nq], in_=ps[:, bank, 0:nq], func=AF.Exp),
                          reads=[self.bank[bank]], writes=[pe32_b])
                    gh = g * 16 + 2 * p + e_
                    e_ap = self.E[:, gh, qi0:qi0 + nq]
                    Sx.op("dve", lambda e: e.tensor_tensor(out=PT[:, 0:nq], in0=pe32[:, 0:nq], in1=e_ap, op=ALU.mult),
                          reads=[pe32_b, self.E_b], writes=[PT_b])
                    return (e_, ci, res, qlo, qhi, nq, PT, PT_b, bank)

                def pv_stage(stg):
                    e_, ci, res, qlo, qhi, nq, PT, PT_b, bank = stg
                    ob = bank + 2
                    v_ap = self.Vaug[e_][:, ci, :]
                    Sx.op("pe", lambda pe: pe.matmul(ps[:, ob, 0:nq], lhsT=v_ap, rhs=PT[:, 0:nq], start=True, stop=True),
                          reads=[self.Vaug_b[e_], PT_b], writes=[self.bank[ob]])
                    a3 = self.acc[e_].rearrange("p (t r) -> p t r", r=r)
                    Sx.op("dve", lambda e: e.tensor_tensor(out=a3[:, qlo:qhi, res], in0=a3[:, qlo:qhi, res],
                                                           in1=ps[:, ob, 0:nq], op=ALU.add),
                          reads=[self.bank[ob], self.acc_b[e_]], writes=[self.acc_b[e_]])

                for (e_, ci) in work:
                    stg = st_stage(e_, ci)
                    if pend is not None:
                        pv_stage(pend)
                    pend = stg
                pv_stage(pend)
            den = self.den
            Sx.dma("sp", lambda e: e.dma_start(out=den[0:64, :], in_=self.acc[0][64:128, :]),
                   reads=[self.acc_b[0]], writes=[self.den_b[0]])
            Sx.dma("sp", lambda e: e.dma_start(out=den[64:128, :], in_=self.acc[1][0:64, :]),
                   reads=[self.acc_b[1]], writes=[self.den_b[1]])
            Sx.op("dve", lambda e: e.reciprocal(out=den, in_=den), reads=self.den_b, writes=self.den_b)
            Sx.op("dve", lambda e, p=p: e.tensor_tensor(out=oT[0:64, p, :], in0=self.acc[0][0:64, :], in1=den[0:64, :],
                                                        op=ALU.mult),
                  reads=[self.acc_b[0]] + self.den_b, writes=[self.oT_b])
            Sx.op("dve", lambda e, p=p: e.tensor_tensor(out=oT[64:128, p, :], in0=self.acc[1][64:128, :],
                                                        in1=den[64:128, :], op=ALU.mult),
                  reads=[self.acc_b[1]] + self.den_b, writes=[self.oT_b])
        slots = [self.w_get(), self.w_get(hold=1)]
        for t in range(16):
            b0 = 2 * (t % 2)

            def mmo(pe, t=t, b0=b0):
                for h in range(2):
                    w3 = slots[h][0].rearrange("p (k n) -> p k n", n=512)
                    for k in range(8):
                        ins = pe.matmul(ps[:, b0 + h, :], lhsT=oT[:, k, t * 128:(t + 1) * 128], rhs=w3[:, k, :],
                                        start=(k == 0), stop=(k == 7))
                return ins
            Sx.op("pe", mmo, reads=[slots[0][1], slots[1][1], self.oT_b], writes=[self.bank[b0], self.bank[b0 + 1]])
            self.postnorm_tile(first, s, t, b0, gset + 1, 1.0)

    def ret_unit(self, layer, s):
        j = layer // 2
        win = self.rwin[j]
        full = lambda sl: sl.rearrange("p (k n) -> p k n", n=512)
        slabs = []
        for h in range(4):
            qsrc = win[:, h * 256:(h + 1) * 256].rearrange("(k p) n -> p k n", p=128)
            ksrc = win[:, 1024 + h * 256:1024 + (h + 1) * 256].rearrange("(k p) n -> p k n", p=128)
            slabs.append([((lambda sl: sl.rearrange("p (k n) -> p k n", n=512)[:, :, 0:256]), qsrc),
                          ((lambda sl: sl.rearrange("p (k n) -> p k n", n=512)[:, :, 256:512]), ksrc)])
            for base in (2048, 4096, 6144):
                src = win[:, base + h * 512: base + (h + 1) * 512].rearrange("(k p) n -> p k n", p=128)
                slabs.append([(full, src)])
        wout = self.rwout[j]
        for dh in range(2):
            for kh in range(2):
                src = wout[kh * 1024:(kh + 1) * 1024, dh * 512:(dh + 1) * 512].rearrange("(k p) n -> p k n", p=128)
                slabs.append([(full, src)])
        first = (layer, 1) == tuple(self.plan[0])
        return dict(kind="ret", layer=layer, s=s, slabs=slabs, run=self.ret_run, first=first)

    def ret_alloc(self):
        A = self.A
        A.push()
        self.cs = A.alloc(2 * S * 4, F32).rearrange("p (a t) -> p a t", t=S)
        self.cs_b = Buf("cs")
        self.QrT = A.alloc(2 * S * 2).rearrange("p (a t) -> p a t", t=S)
        self.KrT = A.alloc(2 * S * 2).rearrange("p (a t) -> p a t", t=S)
        self.QrT_b, self.KrT_b = Buf("QrT"), Buf("KrT")
        self.Krtm = A.alloc(16 * 256 * 2).rearrange("p (c n) -> p c n", n=256)
        self.Krtm_b = Buf("Krtm")
        self.Vtm = A.alloc(16 * 512 * 2).rearrange("p (c n) -> p c n", n=512)
        self.Vtm_b = Buf("Vtm")
        self.YF = A.alloc(16 * 512 * 2).rearrange("p (c n) -> p c n", n=512)
        self.YF_b = Buf("YF")
        self.S32 = A.alloc(1024 * 4, F32)
        self.S32_b = Buf("S32")
        self.Sbf = A.alloc(1024 * 2)
        self.Sbf_b = Buf("Sbf")
        self.tmp_ring = Ring([(A.alloc(2048, F32), Buf(f"tmp{i}")) for i in range(2)])
        self.Qc_ring = Ring([(A.alloc(512).rearrange("p (a t) -> p a t", t=128), Buf(f"Qc{i}")) for i in range(2)])
        self.Kh_ring = Ring([(A.alloc(512), Buf(f"Kh{i}")) for i in range(2)])
        self.AT_ring = Ring([(A.alloc(256), Buf(f"AT{i}")) for i in range(2)])
        self.qs = A.alloc(8 * 128 * 4, F32).rearrange("p (g n) -> p g n", n=128)
        self.Mp = A.alloc(8 * 128 * 4, F32).rearrange("p (g n) -> p g n", n=128)
        self.rc = A.alloc((4 * 128 + 2) * 4, F32)[:, 0:514]
        self.cols = A.alloc(64 * 4, F32)
        self.dec_b = Buf("dec")
        self.rc_b = Buf("rc")
        self.yst_ring = Ring([(A.alloc(1024).rearrange("p (a t) -> p a t", t=128), Buf(f"yst{i}")) for i in range(2)])
        self.ybf_ring = Ring([(A.alloc(1024), Buf(f"ybf{i}")) for i in range(1)])
        self.eg_ring = Ring([(A.alloc(1024), Buf(f"eg{i}")) for i in range(3)])
        A.pop()

    def ret_decay_setup(self, j):
        Sx = self.S
        c = self.cols
        rc = self.rc
        db = self.dec_b
        dl, ee, lg, nlg, gC = c[:, 0:8], c[:, 8:16], c[:, 16:24], c[:, 24:32], c[:, 32:40]
        kd = c[:, 40:48]
        ksc = c[:, 48:56]
        src = self.rdl[j:j + 1, :].partition_broadcast(128)
        Sx.dma("sp", lambda e: e.dma_start(out=dl, in_=src), writes=[db])
        Sx.op("act", lambda e: e.activation(out=ee, in_=dl, func=AF.Exp), reads=[db], writes=[db])
        Sx.op("act", lambda e: e.activation(out=lg, in_=ee, func=AF.Ln, scale=-1.0, bias=1.0), reads=[db], writes=[db])
        Sx.op("act", lambda e: e.activation(out=nlg, in_=lg, func=AF.Copy, scale=-1.0), reads=[db], writes=[db])
        Sx.op("act", lambda e: e.activation(out=gC, in_=lg, func=AF.Exp, scale=128.0), reads=[db], writes=[db])
        for d in range(2):
            for h in range(4):
                i = d * 4 + h
                ramp = rc[:, 256 + d * 128: 256 + (d + 1) * 128]
                colr = rc[:, 512 + d: 513 + d]
                mask = rc[:, d * 128:(d + 1) * 128]
                Sx.op("act", lambda e, i=i, ramp=ramp: e.activation(out=self.qs[:, i, :], in_=ramp, func=AF.Exp,
                                                                    scale=lg[:, i:i + 1]),
                      reads=[db, self.rc_b], writes=[db])
                Sx.op("act", lambda e, i=i, colr=colr: e.activation(out=ksc[:, i:i + 1], in_=colr, func=AF.Exp,
                                                                    scale=nlg[:, i:i + 1]),
                      reads=[db, self.rc_b], writes=[db])
                Sx.op("dve", lambda e, i=i, mask=mask: e.tensor_scalar(out=self.Mp[:, i, :], in0=mask,
                                                                       scalar1=ksc[:, i:i + 1], scalar2=0.0625,
                                                                       op0=ALU.mult, op1=ALU.mult),
                      reads=[db, self.rc_b], writes=[db])
                Sx.op("dve", lambda e, i=i: e.tensor_scalar(out=kd[:, i:i + 1], in0=ksc[:, i:i + 1],
                                                            scalar1=gC[:, i:i + 1], scalar2=0.0625,
                                                            op0=ALU.mult, op1=ALU.mult),
                      reads=[db], writes=[db])

    def ret_run(self, u):
        Sx = self.S
        layer, s, first = u["layer"], u["s"], u["first"]
        j = layer // 2
        if getattr(self, "phase", None) != "ret":
            self.phase_switch("ret")
        gset = 2 * ((layer * 3 + 1) % 2)
        if s == 0:
            self.load_gain(gset, layer, 2)
            self.load_gain(gset + 1, layer, 3)
            self.ret_decay_setup(j)
        self.prenorm_tiles(first, s, range(16), gset)
        hT, ps = self.hT, self.ps
        cs, QrT, KrT, Krtm, Vtm, YF = self.cs, self.QrT, self.KrT, self.Krtm, self.Vtm, self.YF
        S32, Sbf = self.S32, self.Sbf
        S32_3 = S32.rearrange("p (a n) -> p a n", n=512)
        Sbf_3 = Sbf.rearrange("p (a n) -> p a n", n=512)
        c_ = self.cols
        gC, kd = c_[:, 32:40], c_[:, 40:48]
        ident = self.ident
        db = self.dec_b
        nq = 0
        for h in range(4):
            slot_qk, slot_qk_b = self.w_get()
            wqk = slot_qk.rearrange("p (k n) -> p k n", n=512)
            for which, dstT, dst_b in ((0, QrT, self.QrT_b), (1, KrT, self.KrT_b)):
                for tb in range(4):
                    cols = slice(tb * 512, (tb + 1) * 512)

                    def mmq(pe, wqk=wqk, off=which * 256, cols=cols):
                        for dc in range(2):
                            for k in range(8):
                                ins = pe.matmul(ps[:, dc, :], lhsT=wqk[:, k, off + dc * 128: off + (dc + 1) * 128],
                                                rhs=hT[:, k, cols], start=(k == 0), stop=(k == 7))
                        return ins
                    Sx.op("pe", mmq, reads=[slot_qk_b] + self.hT_b[tb * 4:tb * 4 + 4], writes=[self.bank[0], self.bank[1]])
                    ta, ta_b = self.tmp_ring.next()
                    tb_, tb_b = self.tmp_ring.next()
                    t1p, t2p = ps[:, 0, :], ps[:, 1, :]
                    cosb, sinb = cs[:, 0, cols], cs[:, 1, cols]
                    rd = [self.bank[0], self.bank[1], self.cs_b]
                    Sx.op("dve", lambda e, ta=ta, t1p=t1p, cosb=cosb: e.tensor_tensor(out=ta, in0=t1p, in1=cosb, op=ALU.mult),
                          reads=rd, writes=[ta_b])
                    Sx.op("dve", lambda e, tb_=tb_, t2p=t2p, sinb=sinb: e.tensor_tensor(out=tb_, in0=t2p, in1=sinb, op=ALU.mult),
                          reads=rd, writes=[tb_b])
                    Sx.op("dve", lambda e, ta=ta, tb_=tb_, dstT=dstT, cols=cols: e.tensor_tensor(
                        out=dstT[:, 0, cols], in0=ta, in1=tb_, op=ALU.subtract), reads=[ta_b, tb_b], writes=[dst_b])
                    Sx.op("dve", lambda e, ta=ta, t1p=t1p, sinb=sinb: e.tensor_tensor(out=ta, in0=t1p, in1=sinb, op=ALU.mult),
                          reads=rd, writes=[ta_b])
                    Sx.op("dve", lambda e, tb_=tb_, t2p=t2p, cosb=cosb: e.tensor_tensor(out=tb_, in0=t2p, in1=cosb, op=ALU.mult),
                          reads=rd, writes=[tb_b])
                    Sx.op("dve", lambda e, ta=ta, tb_=tb_, dstT=dstT, cols=cols: e.tensor_tensor(
                        out=dstT[:, 1, cols], in0=ta, in1=tb_, op=ALU.add), reads=[ta_b, tb_b], writes=[dst_b])
            pK = ps[:, 7, 0:512].bitcast(BF16).rearrange("p (c n) -> p c n", n=256)
            for c4 in range(4):
                def trk(pe, c4=c4):
                    for cl in range(4):
                        c = c4 * 4 + cl
                        for dc in range(2):
                            ins = pe.transpose(pK[:, cl, dc * 128:(dc + 1) * 128], KrT[:, dc, c * 128:(c + 1) * 128], ident)
                    return ins
                Sx.op("pe", trk, reads=[self.KrT_b, self.ident_b], writes=[self.bank[7]])
                Sx.op("act", lambda e, c4=c4: e.activation(out=Krtm[:, c4 * 4:c4 * 4 + 4, :], in_=pK, func=AF.Copy),
                      reads=[self.bank[7]], writes=[self.Krtm_b])
            slot_v, slot_v_b = self.w_get()
            wv = slot_v.rearrange("p (k n) -> p k n", n=512)
            for c in range(16):
                bank = 2 + c % 2

                def mmv(pe, wv=wv, c=c, bank=bank):
                    for k in range(8):
                        ins = pe.matmul(ps[:, bank, :], lhsT=hT[:, k, c * 128:(c + 1) * 128], rhs=wv[:, k, :],
                                        start=(k == 0), stop=(k == 7))
                    return ins
                Sx.op("pe", mmv, reads=[slot_v_b, self.hT_b[c]], writes=[self.bank[bank]])
                Sx.op("act", lambda e, c=c, bank=bank: e.activation(out=Vtm[:, c, :], in_=ps[:, bank, :], func=AF.Copy),
                      reads=[self.bank[bank]], writes=[self.Vtm_b])
            for d in range(2):
                i8 = d * 4 + h
                slot_g, slot_g_b = self.w_get()
                wgt = slot_g.rearrange("p (k n) -> p k n", n=512)
                Sx.op("dve", lambda e: e.memset(S32, 0.0), writes=[self.S32_b])
                Sx.op("dve", lambda e: e.memset(Sbf, 0.0), writes=[self.Sbf_b])
                order = list(range(16)) if d == 0 else list(range(15, -1, -1))
                stt = {}

                def P0(k, d=d, i8=i8, wgt=wgt, slot_g_b=slot_g_b, order=order, stt=stt):
                    c = order[k]
                    tok = slice(c * 128, (c + 1) * 128)
                    Qc, Qc_b = self.Qc_ring.next()
                    for dc in range(2):
                        Sx.op("dve", lambda e, dc=dc: e.tensor_tensor(out=Qc[:, dc, :], in0=QrT[:, dc, tok], in1=self.qs[:, i8, :],
                                                                      op=ALU.mult),
                              reads=[self.QrT_b, db], writes=[Qc_b])
                    Kh, Kh_b = self.Kh_ring.next()
                    Sx.op("dve", lambda e: e.tensor_scalar(out=Kh, in0=Krtm[:, c, :], scalar1=kd[:, i8:i8 + 1], scalar2=None,
                                                           op0=ALU.mult),
                          reads=[self.Krtm_b, db], writes=[Kh_b])

                    def mma(pe):
                        for dc in range(2):
                            ins = pe.matmul(ps[:, 0, 0:128], lhsT=KrT[:, dc, tok], rhs=Qc[:, dc, :], start=(dc == 0), stop=(dc == 1))
                        return ins
                    Sx.op("pe", mma, reads=[self.KrT_b, Qc_b], writes=[self.bank[0]])
                    gb_ = 2 + k % 2

                    def mmg_(pe):
                        for kk in range(8):
                            ins = pe.matmul(ps[:, gb_, :], lhsT=hT[:, kk, tok], rhs=wgt[:, kk, :], start=(kk == 0), stop=(kk == 7))
                        return ins
                    Sx.op("pe", mmg_, reads=[slot_g_b, self.hT_b[c]], writes=[self.bank[gb_]])

                    def mms(pe):
                        for dc in range(2):
                            ins = pe.matmul(ps[:, 4 + dc, :], lhsT=Kh[:, dc * 128:(dc + 1) * 128], rhs=Vtm[:, c, :], start=True, stop=True)
                        return ins
                    Sx.op("pe", mms, reads=[Kh_b, self.Vtm_b], writes=[self.bank[4], self.bank[5]])
                    stt[k] = dict(c=c, Qc=Qc, Qc_b=Qc_b, gb_=gb_)

                def P1(k, d=d, i8=i8, stt=stt):
                    z = stt[k]
                    c, Qc, Qc_b, gb_ = z["c"], z["Qc"], z["Qc_b"], z["gb_"]
                    AT, AT_b = self.AT_ring.next()
                    Sx.op("dve", lambda e: e.tensor_tensor(out=AT, in0=ps[:, 0, 0:128], in1=self.Mp[:, i8, :], op=ALU.mult),
                          reads=[self.bank[0], db], writes=[AT_b])
                    yb_ = 1 if k % 2 == 0 else 6

                    def mmy(pe):
                        pe.matmul(ps[:, yb_, :], lhsT=AT, rhs=Vtm[:, c, :], start=True, stop=False)
                        for dc in range(2):
                            ins = pe.matmul(ps[:, yb_, :], lhsT=Qc[:, dc, :], rhs=Sbf_3[:, dc, :], start=False, stop=(dc == 1))
                        return ins
                    Sx.op("pe", mmy, reads=[AT_b, Qc_b, self.Vtm_b, self.Sbf_b], writes=[self.bank[yb_]])
                    Sx.op("dve", lambda e: e.scalar_tensor_tensor(out=S32_3, in0=S32_3, scalar=gC[:, i8:i8 + 1],
                                                                  in1=ps[:, 4:6, :], op0=ALU.mult, op1=ALU.add),
                          reads=[self.bank[4], self.bank[5], self.S32_b, db], writes=[self.S32_b])
                    Sx.op("act", lambda e: e.activation(out=Sbf, in_=S32, func=AF.Copy), reads=[self.S32_b], writes=[self.Sbf_b])
                    eg, eg_b = self.eg_ring.next()
                    Sx.op("act", lambda e: e.activation(out=eg, in_=ps[:, gb_, :], func=AF.Silu),
                          reads=[self.bank[gb_]], writes=[eg_b])
                    z.update(yb_=yb_, eg=eg, eg_b=eg_b)

                def P2(k, d=d, h=h, stt=stt):
                    z = stt.pop(k)
                    c, yb_, eg, eg_b = z["c"], z["yb_"], z["eg"], z["eg_b"]
                    st, st_b = self.stat_ring.next()
                    st2, st2_b = self.stat_ring.next()
                    yn, yn_b = self.tmp_ring.next()
                    ypsum = ps[:, yb_, :]
                    Sx.op("act", lambda e: e.activation(out=yn, in_=ypsum, func=AF.Copy, scale=1.0 / 512, accum_out=st2[:, 0:1]),
                          reads=[self.bank[yb_]], writes=[yn_b, st2_b])
                    Sx.op("act", lambda e: e.activation(out=yn, in_=ypsum, func=AF.Square, scale=512.0 ** -0.5, accum_out=st[:, 1:2]),
                          reads=[self.bank[yb_]], writes=[yn_b, st_b])
                    Sx.op("dve", lambda e: e.tensor_tensor(out=st2[:, 1:2], in0=st2[:, 0:1], in1=st2[:, 0:1], op=ALU.mult),
                          reads=[st2_b], writes=[st2_b])
                    Sx.op("dve", lambda e: e.scalar_tensor_tensor(out=st[:, 2:3], in0=st[:, 1:2], scalar=EPS, in1=st2[:, 1:2],
                                                                  op0=ALU.add, op1=ALU.subtract),
                          reads=[st_b, st2_b], writes=[st_b])
                    Sx.op("pool", lambda e: e.tensor_tensor(out=st[:, 3:4], in0=st[:, 2:3], in1=self.mhalf[:, 0:1], op=ALU.pow),
                          reads=[st_b, self.mhalf_b], writes=[st_b])
                    Sx.op("dve", lambda e: e.tensor_scalar(out=yn, in0=ypsum, scalar1=st2[:, 0:1], scalar2=st[:, 3:4],
                                                           op0=ALU.subtract, op1=ALU.mult),
                          reads=[self.bank[yb_], st_b, st2_b], writes=[yn_b])
                    if d == 0:
                        Sx.op("dve", lambda e: e.tensor_tensor(out=YF[:, c, :], in0=yn, in1=eg, op=ALU.mult),
                              reads=[yn_b, eg_b], writes=[self.YF_b])
                    else:
                        Sx.op("dve", lambda e: e.tensor_tensor(out=yn, in0=yn, in1=eg, op=ALU.mult),
                              reads=[yn_b, eg_b], writes=[yn_b])
                        ybf, ybf_b = self.ybf_ring.next()
                        Sx.op("dve", lambda e: e.tensor_tensor(out=ybf, in0=yn, in1=YF[:, c, :], op=ALU.add),
                              reads=[yn_b, self.YF_b], writes=[ybf_b])
                        pY = ps[:, 7, 0:256].bitcast(BF16).rearrange("p (a t) -> p a t", t=128)

                        def try_(pe):
                            for ec in range(4):
                                ins = pe.transpose(pY[:, ec, :], ybf[:, ec * 128:(ec + 1) * 128], ident)
                            return ins
                        Sx.op("pe", try_, reads=[ybf_b, self.ident_b], writes=[self.bank[7]])
                        yst, yst_b = self.yst_ring.next()
                        Sx.op("act", lambda e: e.activation(out=yst, in_=pY, func=AF.Copy), reads=[self.bank[7]], writes=[yst_b])
                        dstd = self.yT_d[s, :, 4 * h:4 * h + 4, c * 128:(c + 1) * 128]
                        Sx.dma("sp", lambda e: e.dma_start(out=dstd, in_=yst), reads=[yst_b], writes=[self.yTd_b[s][c // 4]])

                for it in range(18):
                    if 0 <= it - 1 < 16:
                        P1(it - 1)
                    if it < 16:
                        P0(it)
                    if 0 <= it - 2 < 16:
                        P2(it - 2)
        wsl = [self.w_get(hold=i) for i in range(4)]
        yTl = Vtm
        for tb in range(4):
            srcd = self.yT_d[s, :, :, tb * 512:(tb + 1) * 512]
            Sx.dma("sp", lambda e, srcd=srcd: e.dma_start(out=yTl, in_=srcd), reads=[self.yTd_b[s][tb]], writes=[self.Vtm_b])
            for tt in range(4):
                t = tb * 4 + tt
                b0 = 2 * (t % 2)

                def mmo(pe, tt=tt, b0=b0):
                    for dh in range(2):
                        for kc in range(16):
                            w3 = wsl[dh * 2 + kc // 8][0].rearrange("p (k n) -> p k n", n=512)
                            ins = pe.matmul(ps[:, b0 + dh, :], lhsT=yTl[:, kc, tt * 128:(tt + 1) * 128], rhs=w3[:, kc % 8, :],
                                            start=(kc == 0), stop=(kc == 15))
                    return ins
                Sx.op("pe", mmo, reads=[w_[1] for w_ in wsl] + [self.Vtm_b], writes=[self.bank[b0], self.bank[b0 + 1]])
                self.postnorm_tile(first, s, t, b0, gset + 1, 1.0)

    def phase_switch(self, name):
        fence = {}
        for e in ("pe", "act", "dve", "pool"):
            if self.S.tick[e] > 0:
                fence[e] = self.S.tick[e]
        for q in ("sp", "pool", "act"):
            for i in range(DMA_K):
                if self.S.pool_target[q][i] > 0:
                    fence[("dma", q, i)] = self.S.pool_target[q][i]
        self.phase = name
        if name == "ffn":
            self.ffn_alloc()
            locs = self.aT_b + [self.Wd_b] + [b for _, b in self.sg_ring.items]
        elif name == "attn":
            self.attn_alloc()
            locs = ([self.oT_b, self.qT_b, self.kT_b, self.E_b] + self.acc_b + self.den_b + self.Vaug_b
                    + [b for _, b in self.pe32_ring.items] + [b for _, b in self.PT_ring.items])
        elif name == "ret":
            self.ret_alloc()
            locs = ([self.cs_b, self.QrT_b, self.KrT_b, self.Krtm_b, self.Vtm_b, self.YF_b, self.S32_b, self.Sbf_b,
                     self.dec_b, self.rc_b]
                    + [b for rg in (self.tmp_ring, self.Qc_ring, self.Kh_ring, self.AT_ring, self.yst_ring, self.ybf_ring,
                                    self.eg_ring)
                       for _, b in rg.items])
        for bf in locs:
            bf.r = dict(fence)
        if name == "attn":
            self.attn_setup()
        if name == "ret":
            self.S.dma("sp", lambda e: e.dma_start(out=self.cs, in_=self.cs_d), writes=[self.cs_b])
            self.S.dma("sp", lambda e: e.dma_start(out=self.rc, in_=self.rconst), writes=[self.rc_b])


FULL_PLAN = [(l, sub) for l in range(NL) for sub in range(3)]


def t5_buckets(rel):
    half = 16
    max_exact = 8
    n = np.abs(rel)
    large = max_exact + (np.log(np.maximum(n, 1) / max_exact) / np.log(1024 / max_exact) * (half - max_exact)).astype(np.int64)
    large = np.minimum(large, half - 1)
    return ((rel > 0) * half + np.where(n < max_exact, n, large)).astype(np.int32)


def expand_bias(rel_bias):
    kp = np.arange(128)[:, None]
    qi = np.arange(256)[None, :]
    off = kp - qi + 64
    valid = np.abs(off) <= 64
    out = np.full((128, 48, 256), -30000.0, dtype=np.float32)
    for g, dil in enumerate((1, 4, 16)):
        bk = t5_buckets(np.clip(off, -64, 64) * dil)
        for h in range(16):
            tile = rel_bias[g * 16 + h][bk]
            out[:, g * 16 + h, :] = np.where(valid, tile, np.float32(-30000.0))
    return out


def ret_consts():
    jj = np.arange(128)[:, None]
    ii = np.arange(128)[None, :]
    maskf = (ii >= jj).astype(np.float32)
    maskb = (jj >= ii).astype(np.float32)
    rampf = np.broadcast_to((ii + 1).astype(np.float32), (128, 128))
    rampb = np.broadcast_to((128 - ii).astype(np.float32), (128, 128))
    colr = np.concatenate([(jj + 1), (128 - jj)], axis=1).astype(np.float32)
    rconst = np.ascontiguousarray(np.concatenate([maskf, maskb, rampf, rampb, colr], axis=1), dtype=np.float32)
    inv_freq = (1.0 / (np.float32(10000.0) ** np.linspace(0.0, 1.0, 128, dtype=np.float32))).astype(np.float32)
    ang = (np.arange(S, dtype=np.float32)[None, :] * inv_freq[:, None]).astype(np.float32)
    cossin = np.stack([np.cos(ang), np.sin(ang)], axis=1).astype(np.float32)
    return rconst, np.ascontiguousarray(cossin)


def run_plan(inputs, plan, nseq, ncores, trace=False, debug_out=None):
    prog = Prog(nseq, plan, debug_out)
    nc = prog.build()
    x = np.ascontiguousarray(inputs["x"], dtype=np.float32)
    ident = np.eye(128, dtype=np.float32)
    biasexp = expand_bias(np.asarray(inputs["rel_bias"], dtype=np.float32))
    rconst, cossin = ret_consts()
    in_maps = []
    for c in range(ncores):
        m = {"x": x[c * nseq:(c + 1) * nseq].reshape(nseq * S, D),
             "norm_gains": np.ascontiguousarray(inputs["norm_gains"], dtype=np.float32),
             "ffn_w_gate": np.ascontiguousarray(inputs["ffn_w_gate"], dtype=np.float32),
             "ffn_w_up": np.ascontiguousarray(inputs["ffn_w_up"], dtype=np.float32),
             "ffn_w_down": np.ascontiguousarray(inputs["ffn_w_down"], dtype=np.float32),
             "ident": ident,
             "attn_w_in": np.ascontiguousarray(inputs["attn_w_in"], dtype=np.float32),
             "attn_w_out": np.ascontiguousarray(inputs["attn_w_out"], dtype=np.float32),
             "biasexp": biasexp,
             "ret_w_in": np.ascontiguousarray(inputs["ret_w_in"], dtype=np.float32),
             "ret_w_out": np.ascontiguousarray(inputs["ret_w_out"], dtype=np.float32),
             "ret_decay_logit": np.ascontiguousarray(inputs["ret_decay_logit"], dtype=np.float32).reshape(2, 8),
             "rconst": rconst, "cossin": cossin}
        in_maps.append(m)
    res = run_bass_kernel_spmd(nc, in_maps, core_ids=list(range(ncores)), trace=trace)
    out = np.stack([r["y"].reshape(nseq, S, D) for r in res.results], axis=0).reshape(ncores * nseq, S, D)
    if debug_out is not None:
        return out, res, {k: res.results[0][k] for k in prog.dbg_names}
    return out, res


def kernel(**inputs):
    out, _ = run_plan(inputs, FULL_PLAN, 2, NCORES)
    return out.astype(np.float32)
```
